# Optimizing a Trainium2 kernel written in Bass

```python
import jax
import jax.numpy as jnp
from jax import lax
import numpy as np

D_MODEL = 1024
BATCH = 8
SEQ = 2048
DEPTH = 1

D_MIX = D_MODEL
D_RWKV = D_MIX // 2
D_HGRN = D_MIX - D_RWKV
RWKV_HEAD = 64
RWKV_HEADS = D_RWKV // RWKV_HEAD
RANK_W = 64
RANK_A = 64
RANK_G = 128
HGRN_HEAD = 128
HGRN_HEADS = D_HGRN // HGRN_HEAD
CHUNK = 64
CONV_W = 4
N_GROUPS = 4
EXPERTS_PER_GROUP = 8
N_EXPERTS = N_GROUPS * EXPERTS_PER_GROUP
TOP_K = 2
D_EXPERT = 512
RMS_EPS = 1e-6
GN_EPS = 64e-5
L2_EPS = 1e-12
D_RWKV_IN = 3 * D_RWKV + RANK_W + RANK_A + RANK_G
D_HGRN_IN = 4 * D_HGRN
D_IN = D_RWKV_IN + D_HGRN_IN

kernel_name = 'hybrid_rwkv7_hgrn2_hmoe'


def rms_norm(x, gain):
    xf = x.astype(jnp.float32)
    y = xf * lax.rsqrt(jnp.mean(xf * xf, axis=-1, keepdims=True) + RMS_EPS)
    return (y * gain.astype(jnp.float32)).astype(x.dtype)


def token_shift(x):
    return jnp.pad(x, ((0, 0), (1, 0), (0, 0)))[:, :-1]


def causal_dwconv(x, w):
    return lax.conv_general_dilated(
        x, w[:, None, :].astype(x.dtype), window_strides=(1,),
        padding=[(CONV_W - 1, 0)], dimension_numbers=('NWC', 'WIO', 'NWC'),
        feature_group_count=x.shape[-1])


def rwkv7_mixer(p, mu, w0, w_up, a0, a_up, g_up, k_k, k_a, r_k, ln_w, ln_b):
    B, S, _ = p.shape
    f32 = jnp.float32
    p = p.astype(f32)
    p = p + mu * (token_shift(p) - p)
    splits = [D_RWKV, 2 * D_RWKV, 3 * D_RWKV, 3 * D_RWKV + RANK_W, 3 * D_RWKV + RANK_W + RANK_A]
    r, k, v, dw, da, dg = jnp.split(p, splits, axis=-1)
    w = -jax.nn.softplus(-(w0 + jnp.tanh(dw) @ w_up)) - 0.5
    decay = jnp.exp(-jnp.exp(w))
    a = jax.nn.sigmoid(a0 + da @ a_up)
    g = jax.nn.sigmoid(dg) @ g_up
    hs = lambda t: t.reshape(B, S, RWKV_HEADS, RWKV_HEAD)
    kk = hs(k * k_k)
    kk = kk / jnp.maximum(jnp.linalg.norm(kk, axis=-1, keepdims=True), L2_EPS)
    k = k * (1.0 + (a - 1.0) * k_a)
    r, k, v, decay, a = hs(r), hs(k), hs(v), hs(decay), hs(a)

    def step(state, inp):
        r_t, w_t, k_t, v_t, kk_t, a_t = inp
        sa = jnp.einsum('bhvk,bhk->bhv', state, -kk_t)
        state = (state * w_t[:, :, None, :]
                 + sa[..., None] * (kk_t * a_t)[:, :, None, :]
                 + v_t[..., None] * k_t[:, :, None, :])
        return state, jnp.einsum('bhvk,bhk->bhv', state, r_t)

    xs = tuple(jnp.moveaxis(t, 1, 0) for t in (r, decay, k, v, kk, a))
    s0 = jnp.zeros((B, RWKV_HEADS, RWKV_HEAD, RWKV_HEAD), f32)
    _, y = lax.scan(step, s0, xs)
    y = jnp.moveaxis(y, 0, 1)
    mean = jnp.mean(y, axis=-1, keepdims=True)
    var = jnp.mean(jnp.square(y - mean), axis=-1, keepdims=True)
    y = ((y - mean) * lax.rsqrt(var + GN_EPS) * ln_w.reshape(RWKV_HEADS, RWKV_HEAD)
         + ln_b.reshape(RWKV_HEADS, RWKV_HEAD))
    y = y + jnp.sum(r * k * r_k, axis=-1, keepdims=True) * v
    return y.reshape(B, S, D_RWKV) * g


def hgrn2_mixer(p, conv_w, lb, norm_g):
    B, S, _ = p.shape
    f32 = jnp.float32
    qfi = causal_dwconv(p[..., :3 * D_HGRN], conv_w).astype(f32)
    q, f, i = jnp.split(qfi, 3, axis=-1)
    g = p[..., 3 * D_HGRN:].astype(f32)
    q = jax.nn.silu(q)
    log_f = jnp.logaddexp(jnp.log(lb), jnp.log1p(-lb) + jax.nn.log_sigmoid(f))
    k = (1.0 - lb) * jax.nn.sigmoid(-f)
    n_chunks = S // CHUNK

    def chunks(t):
        return t.reshape(B, n_chunks, CHUNK, HGRN_HEADS, HGRN_HEAD).transpose(1, 0, 3, 2, 4)

    causal = jnp.tril(jnp.ones((CHUNK, CHUNK), dtype=bool))

    def chunk_step(state, inp):
        q_c, k_c, v_c, lf_c = inp
        b = jnp.cumsum(lf_c, axis=2)
        diff = b[:, :, :, None, :] - b[:, :, None, :, :]
        decay = jnp.exp(jnp.where(causal[:, :, None], diff, -jnp.inf))
        scores = jnp.einsum('bhtk,bhsk,bhtsk->bhts', q_c, k_c, decay)
        o = (jnp.einsum('bhts,bhsv->bhtv', scores, v_c)
             + jnp.einsum('bhtk,bhkv->bhtv', q_c * jnp.exp(b), state))
        b_last = b[:, :, -1:, :]
        state = (jnp.exp(b_last[:, :, 0, :, None]) * state
                 + jnp.einsum('bhsk,bhsv->bhkv', k_c * jnp.exp(b_last - b), v_c))
        return state, o

    s0 = jnp.zeros((B, HGRN_HEADS, HGRN_HEAD, HGRN_HEAD), f32)
    _, o = lax.scan(chunk_step, s0, (chunks(q), chunks(k), chunks(i), chunks(log_f)))
    o = o.transpose(1, 0, 3, 2, 4).reshape(B, S, HGRN_HEADS, HGRN_HEAD)
    o = o * lax.rsqrt(jnp.mean(o * o, axis=-1, keepdims=True) + RMS_EPS) * norm_g.reshape(HGRN_HEADS, HGRN_HEAD)
    return o.reshape(B, S, D_HGRN) * jax.nn.silu(g)


def hier_moe(x, wg, bg, we, be, w_gate, w_up, w_down):
    B, S, D = x.shape
    f32 = jnp.float32
    t = x.reshape(B * S, D)
    gp = jax.nn.softmax((t @ wg + bg).astype(f32), axis=-1)
    g_w, g_idx = lax.top_k(gp, 1)
    el = (t @ we + be).astype(f32).reshape(-1, N_GROUPS, EXPERTS_PER_GROUP)
    el = jnp.take_along_axis(el, g_idx[:, :, None], axis=1)[:, 0]
    e_w, e_idx = lax.top_k(jax.nn.softmax(el, axis=-1), TOP_K)
    e_w = e_w / jnp.sum(e_w, axis=-1, keepdims=True)
    ids = g_idx * EXPERTS_PER_GROUP + e_idx
    gates = jnp.sum(jax.nn.one_hot(ids, N_EXPERTS, dtype=f32) * (g_w * e_w)[..., None], axis=1)
    y = jnp.zeros((B * S, D), f32)
    for e in range(N_EXPERTS):
        h = jax.nn.silu(t @ w_gate[e]) * (t @ w_up[e])
        y = y + gates[:, e:e + 1] * (h @ w_down[e])
    return y.reshape(B, S, D).astype(x.dtype)


def setup_inputs(seed: int = 0) -> dict:
    key = jax.random.key(seed)
    keys = jax.random.split(key, 32)
    counter = [0]

    def nxt():
        k = keys[counter[0]]
        counter[0] += 1
        return k

    def nrm(shape, scale):
        return scale * jax.random.normal(nxt(), shape, jnp.float32)

    L = DEPTH
    return {
        'x': nrm((BATCH, SEQ, D_MODEL), 1.0),
        'norm1_g': 1.0 + nrm((L, D_MODEL), 0.02),
        'w_in': nrm((L, D_MODEL, D_IN), D_MODEL ** -0.5),
        'rwkv_mu': jax.random.uniform(nxt(), (L, D_RWKV_IN), jnp.float32),
        'rwkv_w0': jax.random.uniform(nxt(), (L, D_RWKV), jnp.float32, -6.5, -1.0),
        'rwkv_w_up': nrm((L, RANK_W, D_RWKV), 0.1 * RANK_W ** -0.5),
        'rwkv_a0': nrm((L, D_RWKV), 0.1),
        'rwkv_a_up': nrm((L, RANK_A, D_RWKV), 0.1 * RANK_A ** -0.5),
        'rwkv_g_up': nrm((L, RANK_G, D_RWKV), RANK_G ** -0.5),
        'rwkv_k_k': 0.85 + nrm((L, D_RWKV), 0.02),
        'rwkv_k_a': 1.0 + nrm((L, D_RWKV), 0.02),
        'rwkv_r_k': nrm((L, RWKV_HEADS, RWKV_HEAD), 0.1),
        'rwkv_ln_w': 1.0 + nrm((L, D_RWKV), 0.02),
        'rwkv_ln_b': nrm((L, D_RWKV), 0.02),
        'hgrn_conv_w': nrm((L, CONV_W, 3 * D_HGRN), CONV_W ** -0.5),
        'hgrn_lb_logits': nrm((L + 1, D_HGRN), 0.1),
        'hgrn_norm_g': 1.0 + nrm((L, D_HGRN), 0.02),
        'w_out': nrm((L, D_MIX, D_MODEL), D_MIX ** -0.5),
        'norm2_g': 1.0 + nrm((L, D_MODEL), 0.02),
        'router_g_w': nrm((L, D_MODEL, N_GROUPS), D_MODEL ** -0.5),
        'router_g_b': nrm((L, N_GROUPS), 0.01),
        'router_e_w': nrm((L, D_MODEL, N_EXPERTS), D_MODEL ** -0.5),
        'router_e_b': nrm((L, N_EXPERTS), 0.01),
        'exp_w_gate': nrm((L, N_EXPERTS, D_MODEL, D_EXPERT), D_MODEL ** -0.5),
        'exp_w_up': nrm((L, N_EXPERTS, D_MODEL, D_EXPERT), D_MODEL ** -0.5),
        'exp_w_down': nrm((L, N_EXPERTS, D_EXPERT, D_MODEL), D_EXPERT ** -0.5),
        'final_norm_g': 1.0 + nrm((D_MODEL,), 0.02),
    }


def reference(x, norm1_g, w_in, rwkv_mu, rwkv_w0, rwkv_w_up, rwkv_a0, rwkv_a_up, rwkv_g_up,
              rwkv_k_k, rwkv_k_a, rwkv_r_k, rwkv_ln_w, rwkv_ln_b, hgrn_conv_w, hgrn_lb_logits,
              hgrn_norm_g, w_out, norm2_g, router_g_w, router_g_b, router_e_w, router_e_b,
              exp_w_gate, exp_w_up, exp_w_down, final_norm_g):
    lb_all = jnp.cumsum(jax.nn.softmax(hgrn_lb_logits.astype(jnp.float32), axis=0), axis=0)
    h = x
    for l in range(DEPTH):
        n = rms_norm(h, norm1_g[l])
        p = n @ w_in[l]
        ya = rwkv7_mixer(p[..., :D_RWKV_IN], rwkv_mu[l], rwkv_w0[l], rwkv_w_up[l], rwkv_a0[l],
                         rwkv_a_up[l], rwkv_g_up[l], rwkv_k_k[l], rwkv_k_a[l], rwkv_r_k[l],
                         rwkv_ln_w[l], rwkv_ln_b[l])
        yb = hgrn2_mixer(p[..., D_RWKV_IN:], hgrn_conv_w[l], lb_all[l], hgrn_norm_g[l])
        mix = jnp.concatenate([ya, yb], axis=-1).astype(p.dtype)
        h = h + mix @ w_out[l]
        h = h + hier_moe(rms_norm(h, norm2_g[l]), router_g_w[l], router_g_b[l], router_e_w[l],
                         router_e_b[l], exp_w_gate[l], exp_w_up[l], exp_w_down[l])
    return rms_norm(h, final_norm_g)
```

```python
import contextlib
import numpy as np
import concourse.bass as bass
import concourse.mybir as mybir
from concourse.bass_utils import run_bass_kernel_spmd

F32 = mybir.dt.float32
BF16 = mybir.dt.bfloat16
ALU = mybir.AluOpType
AF = mybir.ActivationFunctionType
AX = mybir.AxisListType

D = 1024
NE = 32
DE = 512
C0 = float(np.exp(-0.5))
RMS_EPS = 1e-6
GN_EPS = 64e-5


class Tk:
    def __init__(self, name, handle):
        self.name = name
        self.t = handle
        self.lw = None
        self.rd = []
        self.dsem = None
        self.dcnt = 0
        self.tw = 0.0
        self.tr = 0.0

    def __getitem__(self, idx):
        return self.t[idx]


class _Rec:
    def __init__(self):
        self.call = None

    def __getattr__(self, name):
        def f(*a, **k):
            self.call = (name, a, k)
            return self
        return f


def _record(fn):
    r = _Rec()
    fn(r)
    assert r.call is not None
    return r.call


class Sched:
    ENG = ['tensor', 'vector', 'scalar', 'gpsimd', 'sync']

    def __init__(self, nc, stack):
        self.nc = nc
        self.stack = stack
        self.sem = {e: stack.enter_context(nc.semaphore('s_' + e)) for e in self.ENG}
        self.cnt = {e: 0 for e in self.ENG}
        self.waited = {e: {} for e in self.ENG}
        self.ops = {e: [] for e in self.ENG}
        self.ntile = 0
        self.out_tokens = []
        self.all_dma = {}
        self.psr = 0
        self.rec = None
        self.pspool = (0, 8)
        self.psrs = {}
        self.tm = {e: 0.0 for e in self.ENG}

    def sb(self, name, shape, dtype=F32, stack=None):
        self.ntile += 1
        h = (stack or self.stack).enter_context(self.nc.sbuf_tensor(f'{name}_{self.ntile}', list(shape), dtype))
        return Tk(name, h)

    def ps(self, name, shape, dtype=F32, stack=None):
        self.ntile += 1
        h = (stack or self.stack).enter_context(self.nc.psum_tensor(f'{name}_{self.ntile}', list(shape), dtype))
        return Tk(name, h)

    def _deps(self, eng, reads, writes):
        deps = {}

        def add(tok):
            if tok is None:
                return
            s, v = tok
            if deps.get(s, 0) < v:
                deps[s] = v
        for t in reads:
            add(t.lw)
        for t in writes:
            add(t.lw)
            for r in t.rd:
                add(r)
        waits = []
        for s, v in deps.items():
            if eng == 'tensor' and s is self.sem['tensor']:
                continue
            if self.waited[eng].get(s, 0) >= v:
                continue
            self.waited[eng][s] = v
            waits.append((s, v))
        return waits

    def record(self, f, pspool=(0, 4)):
        assert self.rec is None
        self.rec = []
        old = self.pspool
        self.pspool = pspool
        f()
        out, self.rec = self.rec, None
        self.pspool = old
        return out

    @staticmethod
    def _dur(kind, eng, call):
        if kind == 'dma':
            return 2.0
        name, a, k = call
        out = k.get('out', a[0] if a else None)
        try:
            n = int(np.prod(out.shape[1:]))
        except Exception:
            n = 128
        if eng == 'tensor':
            return 0.07 + n / 2200.0
        if eng == 'vector':
            return 0.10 + n / 960.0
        if eng == 'scalar':
            return 0.25 + n / 1200.0
        return 0.25 + n / 480.0

    def _est(self, item):
        kind, args = item
        eng, call, reads, writes = args[0], args[1], args[2], args[3]
        dep = 0.0
        for t in reads:
            dep = max(dep, t.tw)
        for t in writes:
            dep = max(dep, t.tw, t.tr)
        start = max(self.tm[eng], dep + 0.4)
        return start, start + self._dur(kind, eng, call)

    def _tcommit(self, kind, eng, call, reads, writes):
        start, fin = self._est((kind, (eng, call, reads, writes)))
        self.tm[eng] = start + (0.05 if kind == 'dma' else fin - start)
        for t in reads:
            t.tr = max(t.tr, fin)
        for t in writes:
            t.tw = fin
            t.tr = 0.0

    def run_interleaved(self, la, lb):
        na, nb = len(la), len(lb)
        i = j = 0
        while i < na or j < nb:
            if j >= nb:
                pick_a = True
            elif i >= na:
                pick_a = False
            else:
                pick_a = self._est(la[i])[0] < self._est(lb[j])[0]
            if pick_a:
                kind, args = la[i]; i += 1
            else:
                kind, args = lb[j]; j += 1
            getattr(self, kind)(*args)

    def op(self, eng, fn, reads=(), writes=(), inc=True):
        if self.rec is not None:
            self.rec.append(('op', (eng, _record(fn), tuple(reads), tuple(writes), inc)))
            return
        waits = self._deps(eng, reads, writes)
        call = fn if isinstance(fn, tuple) else _record(fn)
        fn = call
        self._tcommit('op', eng, call, reads, writes)
        tok = (self.sem[eng], self.cnt[eng] + 1)
        if inc:
            self.cnt[eng] += 1
        for t in reads:
            t.rd.append(tok)
        for t in writes:
            t.lw = tok
            t.rd = []
        self.ops[eng].append((fn if isinstance(fn, tuple) else _record(fn), waits, (self.sem[eng], 1) if inc else None))

    def dma(self, eng, fn, reads=(), writes=(), is_output=False):
        if self.rec is not None:
            self.rec.append(('dma', (eng, _record(fn), tuple(reads), tuple(writes), is_output)))
            return
        waits = self._deps(eng, reads, writes)
        call = fn if isinstance(fn, tuple) else _record(fn)
        fn = call
        self._tcommit('dma', eng, call, reads, writes)
        owner = (list(writes) + list(reads))[0]
        if owner.dsem is None:
            self.ntile += 1
            owner.dsem = self.stack.enter_context(self.nc.semaphore(f'd_{owner.name}_{self.ntile}'))
        owner.dcnt += 16
        tok = (owner.dsem, owner.dcnt)
        self.all_dma[id(owner.dsem)] = tok
        for t in reads:
            t.rd.append(tok)
        for t in writes:
            t.lw = tok
            t.rd = []
        if is_output:
            self.out_tokens.append(tok)
        self.ops[eng].append((fn if isinstance(fn, tuple) else _record(fn), waits, (owner.dsem, 16)))

    def barrier(self):
        toks = [(self.sem[e], self.cnt[e]) for e in self.ENG if self.cnt[e] > 0] + list(self.all_dma.values())
        for e in self.ENG:
            waits = []
            for s, v in toks:
                if e == 'tensor' and s is self.sem['tensor']:
                    continue
                if self.waited[e].get(s, 0) >= v:
                    continue
                self.waited[e][s] = v
                waits.append((s, v))
            if waits:
                self.ops[e].append((None, waits, None))

    def finish(self, eng='sync'):
        deps = {}
        for s, v in self.out_tokens:
            deps[s] = max(deps.get(s, 0), v)
        self.ops[eng].append((None, list(deps.items()), None))

    def emit(self):
        nc = self.nc
        with nc.Block() as block:
            def run(engname):
                def body(e):
                    for fn, waits, inc in self.ops[engname]:
                        for s, v in waits:
                            e.wait_ge(s, v)
                        if fn is None:
                            continue
                        name, a, k = fn
                        ins = getattr(e, name)(*a, **k)
                        if inc is not None:
                            ins.then_inc(inc[0], inc[1])
                return body
            block.tensor(run('tensor'))
            block.vector(run('vector'))
            block.scalar(run('scalar'))
            block.gpsimd(run('gpsimd'))
            block.sync(run('sync'))


class Arena:
    def __init__(self, S, nwords):
        self.tk = S.sb('arena', [128, nwords])
        self.n = nwords
        self.off = 0

    def reset(self):
        self.off = 0

    def alloc(self, name, shape, dtype=F32):
        n = int(np.prod(shape[1:]))
        words = n if dtype is F32 else (n + 1) // 2
        assert self.off + words <= self.n, (name, self.off, words, self.n)
        ap = self.tk.t[:, self.off:self.off + words]
        if dtype is not F32:
            ap = ap.bitcast(dtype)
        if len(shape) == 3:
            ap = ap.rearrange('p (a b) -> p a b', b=shape[2])
        self.off += words
        return Tk(name, ap)


ARENA_WORDS = 132 * 256


def build(T, n_exp=NE, do_p1=True, do_p2=True, do_router=True, nb1=None, p1_stage=9):
    NB = T // 128
    nc = bass.Bass("TRN2", target_bir_lowering=False)

    def din(name, shape):
        return nc.dram_tensor(name, list(shape), F32, kind="ExternalInput").ap()
    x_d = din("x", [T, D])
    g1_d = din("norm1_g", [1, D]); g2_d = din("norm2_g", [1, D]); gf_d = din("final_g", [1, D])
    win_d = din("w_in", [D, 3840]); wout_d = din("w_out", [D, D])
    mu_d = din("mu_l", [128, 14])
    rv_d = din("rvec_l", [128, 5, 4])
    waup_d = din("wa_up", [128, 512]); gup_d = din("g_up", [128, 512])
    lnw_d = din("ln_w", [1, 512]); lnb_d = din("ln_b", [1, 512]); hng_d = din("hg_norm", [1, 512])
    cw_d = din("conv_l", [128, 4, 12]); lbl_d = din("lb_l", [128, 2, 4])
    wr_d = din("w_router", [D, 36]); br_d = din("b_router", [1, 36])
    wg_d = din("e_gate", [NE, D, DE]); wu_d = din("e_up", [NE, D, DE]); wd_d = din("e_down", [NE, DE, D])
    out_d = nc.dram_tensor("out", [T, D], F32, kind="ExternalOutput").ap()

    with contextlib.ExitStack() as st:
        S = Sched(nc, st)
        V_, A_, G_, P_ = 'vector', 'scalar', 'gpsimd', 'tensor'

        hb = [S.sb(f'h{b}', [128, D]) for b in range(NB)]
        g1b = S.sb('g1b', [128, D])
        ident = S.sb('ident', [128, 128]); identb = S.sb('identb', [128, 128], BF16)
        MU = S.sb('MU', [128, 128]); MUs = S.sb('MUs', [128, 128]); MLs = S.sb('MLs', [128, 128])
        smask = S.sb('smask', [128, 128]); hsel = S.sb('hsel', [128, 2]); bones = S.sb('bones', [128, 128])
        psb = [S.ps(f'pb{i}', [128, 512]) for i in range(8)]
        AR = Arena(S, ARENA_WORDS)

        def nps():
            lo, hi = S.pspool
            r = S.psrs.get((lo, hi), lo - 1) + 1
            if r >= hi:
                r = lo
            S.psrs[(lo, hi)] = r
            return psb[r]

        def ld(tk, src, eng='sync'):
            S.dma(eng, lambda e: e.dma_start(out=tk[:], in_=src), writes=[tk])

        ld(g1b, g1_d.partition_broadcast(128))
        for b in range(NB):
            S.dma('sync', lambda e, b=b: e.dma_start(out=hb[b][:], in_=x_d[b * 128:(b + 1) * 128, :]), writes=[hb[b]])

        S.op(G_, lambda e: e.memset(ident[:], 0.0), writes=[ident])
        S.op(G_, lambda e: e.affine_select(out=ident[:], in_=ident[:], compare_op=ALU.not_equal, fill=1.0, base=0,
                                           pattern=[[-1, 128]], channel_multiplier=1), reads=[ident], writes=[ident])
        S.op(V_, lambda e: e.tensor_copy(out=identb[:], in_=ident[:]), reads=[ident], writes=[identb])
        for m, cmp_, cm, pat, zb in ((MU, ALU.is_ge, -1, 1, (0, 64)), (MUs, ALU.is_gt, -1, 1, (0, 64)), (MLs, ALU.is_gt, 1, -1, (64, 0))):
            S.op(G_, lambda e, m=m: e.memset(m[:], 1.0), writes=[m])
            S.op(G_, lambda e, m=m, cmp_=cmp_, cm=cm, pat=pat: e.affine_select(
                out=m[:], in_=m[:], compare_op=cmp_, fill=0.0, base=0, pattern=[[pat, 128]], channel_multiplier=cm),
                reads=[m], writes=[m])
            S.op(G_, lambda e, m=m, zb=zb: e.memset(m[zb[0]:zb[0] + 64, zb[1]:zb[1] + 64], 0.0), reads=[m], writes=[m])
        S.op(V_, lambda e: e.memset(smask[:], 1.0), writes=[smask])
        S.op(V_, lambda e: e.memset(smask[:].rearrange('p (c j) -> p c j', j=64)[:, :, 0:1], 0.0), reads=[smask], writes=[smask])
        S.op(V_, lambda e: e.memset(hsel[:], 0.0), writes=[hsel])
        S.op(V_, lambda e: e.memset(hsel[0:64, 0:1], 1.0), reads=[hsel], writes=[hsel])
        S.op(V_, lambda e: e.memset(hsel[64:128, 1:2], 1.0), reads=[hsel], writes=[hsel])
        S.op(V_, lambda e: e.memset(bones[:], 0.0), writes=[bones])
        S.op(V_, lambda e: e.memset(bones[0:64, 0:64], 1.0), reads=[bones], writes=[bones])
        S.op(V_, lambda e: e.memset(bones[64:128, 64:128], 1.0), reads=[bones], writes=[bones])

        def norm_block(xt, gb, outt, sst, eng_sq=A_):
            xtk, xap = xt
            otk, oap = outt
            junk_tk, junk_ap = otk, oap
            s_tk = sst
            S.op(V_, lambda e: e.memset(s_tk[:, 0:1], 0.0), writes=[s_tk])
            S.op(A_, lambda e: e.activation(out=junk_ap, in_=xap, func=AF.Square, accum_out=s_tk[:, 0:1]),
                 reads=[xtk, s_tk], writes=[junk_tk, s_tk])
            S.op(V_, lambda e: e.tensor_scalar(out=s_tk[:, 1:2], in0=s_tk[:, 0:1], scalar1=1.0 / D, scalar2=RMS_EPS,
                                               op0=ALU.mult, op1=ALU.add), reads=[s_tk], writes=[s_tk])
            S.op(A_, lambda e: e.activation(out=s_tk[:, 1:2], in_=s_tk[:, 1:2], func=AF.Sqrt), reads=[s_tk], writes=[s_tk])
            S.op(V_, lambda e: e.reciprocal(out=s_tk[:, 1:2], in_=s_tk[:, 1:2]), reads=[s_tk], writes=[s_tk])
            S.op(V_, lambda e: e.scalar_tensor_tensor(out=oap, in0=xap, scalar=s_tk[:, 1:2], in1=gb[:],
                                                      op0=ALU.mult, op1=ALU.mult), reads=[xtk, s_tk, gb], writes=[otk])

        def transpose_to(dst_tk, dst_ap_fn, src_tk, src_aps, idt, dtype, evac_eng):
            n = len(src_aps)
            pb = nps()
            if dtype is BF16:
                pv = pb[:].bitcast(BF16)
            else:
                pv = pb[:]
            for i, sap in enumerate(src_aps):
                S.op(P_, lambda e, i=i, sap=sap: e.transpose(pv[:, i * 128:(i + 1) * 128], sap, idt[:]),
                     reads=[src_tk, idt], writes=[pb], inc=(i == n - 1))
            src = pv[:, 0:n * 128].rearrange('p (n j) -> p n j', j=128)
            if evac_eng == A_:
                S.op(A_, lambda e: e.copy(out=dst_ap_fn(n), in_=src), reads=[pb], writes=[dst_tk])
            else:
                S.op(evac_eng, lambda e: e.tensor_copy(out=dst_ap_fn(n), in_=src), reads=[pb], writes=[dst_tk])

        def load_norm_transpose(b, xts, nb_t, nT, sst, gb):
            if xts is None:
                xt = hb[b]
            else:
                xt = xts[b % 2]
                S.dma('sync', lambda e: e.dma_start(out=xt[:], in_=x_d[b * 128:(b + 1) * 128, :]), writes=[xt])
            norm_block((xt, xt[:]), gb, (nb_t, nb_t[:]), sst)
            for half in range(2):
                transpose_to(nT, lambda n, half=half: nT[:, half * 4:half * 4 + 4, :], nb_t,
                             [nb_t[:, (half * 4 + i) * 128:(half * 4 + i + 1) * 128] for i in range(4)], identb, BF16,
                             A_ if half == 0 else V_)
            return xt

        def outproj_add(b, y_tk, y_ap, wo, first, xt, scr_bf, yT):
            S.op(V_, lambda e: e.tensor_copy(out=scr_bf[:], in_=y_ap), reads=[y_tk], writes=[scr_bf])
            transpose_to(yT, lambda n: yT[:, 0:4, :], scr_bf, [scr_bf[:, i * 128:(i + 1) * 128] for i in range(4)], identb, BF16, A_)
            for half in range(2):
                pb = nps()
                for k in range(4):
                    S.op(P_, lambda e, k=k, half=half, pb=pb: e.matmul(pb[:, :], lhsT=yT[:, k, :], rhs=wo[:, k, half * 512:(half + 1) * 512],
                                                                      start=(k == 0), stop=(k == 3)),
                         reads=[yT, wo], writes=[pb], inc=(k == 3))
                hap = hb[b][:, half * 512:(half + 1) * 512]
                S.op(V_, lambda e, pb=pb, hap=hap: e.tensor_tensor(out=hap, in0=pb[:, :], in1=hap, op=ALU.add),
                     reads=[pb, hb[b]], writes=[hb[b]])

        if True:
            sb = AR.alloc
            wr_in = sb('wr_in', [128, 8, 1792], BF16)
            wo1 = sb('wo1', [128, 4, D], BF16)
            S.dma(G_, lambda e: e.dma_start(out=wr_in[:], in_=win_d[:, 0:1792].rearrange('(k p) n -> p k n', p=128)), writes=[wr_in])
            S.dma(G_, lambda e: e.dma_start(out=wo1[:], in_=wout_d[0:512, :].rearrange('(k p) n -> p k n', p=128)), writes=[wo1])
            mu_t = sb('mu', [128, 14]); rv = sb('rv', [128, 5, 4]); omka = sb('omka', [128, 4])
            waup = sb('waup', [128, 512]); gup = sb('gup', [128, 512])
            lnw = sb('lnw', [128, 512]); lnb = sb('lnb', [128, 512])
            ld(mu_t, mu_d); ld(rv, rv_d); ld(waup, waup_d); ld(gup, gup_d)
            ld(lnw, lnw_d.partition_broadcast(128)); ld(lnb, lnb_d.partition_broadcast(128))
            S.op(V_, lambda e: e.tensor_scalar(out=omka[:], in0=rv[:, 3, :], scalar1=-1.0, scalar2=1.0, op0=ALU.mult, op1=ALU.add),
                 reads=[rv], writes=[omka])
            nb_t = sb('nb', [128, D], BF16); nT = sb('nT', [128, 8, 128], BF16)
            sst = sb('sst', [128, 2])
            pT1 = sb('pT', [128, 14, 129])
            pm = sb('pm', [128, 14, 128])
            th = sb('th', [128, 128]); sg = sb('sg', [128, 128])
            sw = sb('sw', [128, 4, 128]); cum = sb('cum', [128, 4, 128])
            E3 = sb('E3', [128, 4, 128]); aT = sb('aT', [128, 4, 128])
            kk = sb('kk', [128, 4, 128]); t4 = sb('t4', [128, 4, 128])
            SD = BF16
            bg = sb('bg', [128, 4, 128], SD); kg = sb('kg', [128, 4, 128], SD)
            E1 = sb('E1', [128, 4, 128]); E2 = sb('E2', [128, 4, 128])
            kmod = sw; rk = cum
            ifc1 = [(sb('ag', [128, 4, 128], SD), sb('rg', [128, 4, 128], SD), sb('Vt', [128, 512]), sb('Vtb', [128, 512], SD),
                     sb('bgT', [128, 512], SD), sb('kgT', [128, 512], SD), sb('gC', [128, 4, 2]), sb('gt', [128, 512]), sb('bsum', [128, 8]),
                     sb('Aak', [128, 8, 128], SD), sb('Arb', [128, 8, 128], SD), sb('Ark', [128, 8, 128], SD))
                    for _ in range(2)]
            Pm = [sb('P0', [128, 8, 128], SD), sb('P1', [128, 8, 128], SD)]
            PTm = [sb('PT0', [128, 8, 128], SD), sb('PT1', [128, 8, 128], SD)]
            Nm = Pm[1]; NTm = PTm[1]
            Rm1 = sb('R', [128, 8, 128], SD)
            ST = sb('ST', [128, 4, 64]); STb = sb('STb', [128, 4, 128], SD); xs = sb('xs', [128, 8, 64], SD); us = sb('us', [128, 8, 64], SD)
            ysb = sb('ysb', [128, 8, 64]); yc = sb('yc', [128, 8, 64]); st8 = sb('st8', [128, 4, 8])
            ybf = sb('ybf', [128, 512], BF16); yT = sb('yT', [128, 4, 128], BF16)
            S.op(V_, lambda e: e.memset(ST[:], 0.0), writes=[ST])
            S.op(V_, lambda e: e.memset(STb[:], 0.0), writes=[STb])
            S.op(V_, lambda e: e.memset(pT1[:, :, 0:1], 0.0), writes=[pT1])

            def hrows(h):
                return slice((h % 2) * 64, (h % 2) * 64 + 64)

            def frontA(b):
                xt = load_norm_transpose(b, None, nb_t, nT, sst, g1b)
                cur = pT1
                for g0 in range(0, 14, 4):
                    n = min(4, 14 - g0)
                    pb = nps()
                    for i in range(n):
                        for k in range(8):
                            S.op(P_, lambda e, i=i, k=k, g0=g0, pb=pb: e.matmul(
                                pb[:, i * 128:(i + 1) * 128], lhsT=wr_in[:, k, (g0 + i) * 128:(g0 + i + 1) * 128], rhs=nT[:, k, :],
                                start=(k == 0), stop=(k == 7)), reads=[wr_in, nT], writes=[pb], inc=(k == 7 and i == n - 1))
                    src = pb[:, 0:n * 128].rearrange('p (n j) -> p n j', j=128)
                    if (g0 // 4) % 2 == 0:
                        S.op(A_, lambda e, src=src, g0=g0, n=n, cur=cur: e.copy(out=cur[:, g0:g0 + n, 1:129], in_=src), reads=[pb], writes=[cur])
                    else:
                        S.op(V_, lambda e, src=src, g0=g0, n=n, cur=cur: e.tensor_copy(out=cur[:, g0:g0 + n, 1:129], in_=src), reads=[pb], writes=[cur])
                S.op(V_, lambda e, cur=cur: e.tensor_tensor(out=pm[:], in0=cur[:, :, 0:128], in1=cur[:, :, 1:129], op=ALU.subtract),
                     reads=[cur], writes=[pm])
                S.op(V_, lambda e: e.tensor_tensor(out=pm[:], in0=pm[:], in1=mu_t[:, 0:14].unsqueeze(2).to_broadcast([128, 14, 128]), op=ALU.mult),
                     reads=[pm, mu_t], writes=[pm])
                S.op(V_, lambda e, cur=cur: e.tensor_tensor(out=pm[:], in0=pm[:], in1=cur[:, :, 1:129], op=ALU.add),
                     reads=[pm, cur], writes=[pm])
                S.op(G_, lambda e, cur=cur: e.tensor_copy(out=cur[:, :, 0:1], in_=cur[:, :, 128:129]), reads=[cur], writes=[cur])
                S.op(A_, lambda e: e.activation(out=th[0:64, :], in_=pm[0:64, 12, :], func=AF.Tanh), reads=[pm], writes=[th])
                S.op(A_, lambda e: e.activation(out=sg[:], in_=pm[:, 13, :], func=AF.Sigmoid), reads=[pm], writes=[sg])
                pz = nps()
                for c in range(4):
                    S.op(P_, lambda e, c=c, pz=pz: e.matmul(pz[:, c * 128:(c + 1) * 128], lhsT=waup[0:64, c * 128:(c + 1) * 128], rhs=th[0:64, :],
                                                            start=True, stop=True), reads=[waup, th], writes=[pz], inc=(c == 3))
                pa = nps()
                for c in range(4):
                    S.op(P_, lambda e, c=c, pa=pa: e.matmul(pa[:, c * 128:(c + 1) * 128], lhsT=waup[64:128, c * 128:(c + 1) * 128], rhs=pm[64:128, 12, :],
                                                            start=True, stop=True), reads=[waup, pm], writes=[pa], inc=(c == 3))
                for c in range(4):
                    S.op(A_, lambda e, c=c, pz=pz: e.activation(out=sw[:, c, :], in_=pz[:, c * 128:(c + 1) * 128], func=AF.Sigmoid, bias=rv[:, 0, c:c + 1]),
                         reads=[pz, rv], writes=[sw])
                    S.op(A_, lambda e, c=c, pa=pa: e.activation(out=aT[:, c, :], in_=pa[:, c * 128:(c + 1) * 128], func=AF.Sigmoid, bias=rv[:, 1, c:c + 1]),
                         reads=[pa, rv], writes=[aT])
                for c in range(4):
                    S.op(V_, lambda e, c=c: e.tensor_tensor_scan(out=cum[:, c, :], data0=smask[:], data1=sw[:, c, :], initial=0.0, op0=ALU.mult, op1=ALU.add),
                         reads=[smask, sw], writes=[cum])
                S.op(V_, lambda e: e.tensor_tensor(out=t4[:], in0=cum[:], in1=sw[:], op=ALU.subtract), reads=[cum, sw], writes=[t4])
                S.op(A_, lambda e: e.activation(out=E1[:], in_=cum[:], func=AF.Exp, scale=-C0), reads=[cum], writes=[E1])
                S.op(A_, lambda e: e.activation(out=E3[:], in_=cum[:], func=AF.Exp, scale=C0), reads=[cum], writes=[E3])
                S.op(A_, lambda e: e.activation(out=E2[:], in_=t4[:], func=AF.Exp, scale=-C0), reads=[t4], writes=[E2])
                S.op(V_, lambda e: e.tensor_tensor(out=kk[:], in0=pm[:, 4:8, :], in1=rv[:, 2, :].unsqueeze(2).to_broadcast([128, 4, 128]), op=ALU.mult),
                     reads=[pm, rv], writes=[kk])
                S.op(V_, lambda e: e.tensor_tensor(out=t4[:], in0=kk[:], in1=kk[:], op=ALU.mult), reads=[kk], writes=[t4])
                pn = nps()
                S.op(P_, lambda e, pn=pn: e.matmul(pn[:, :], lhsT=bones[:], rhs=t4[:].rearrange('p c j -> p (c j)'), start=True, stop=True),
                     reads=[bones, t4], writes=[pn])
                S.op(A_, lambda e, pn=pn: e.activation(out=t4[:].rearrange('p c j -> p (c j)'), in_=pn[:, :], func=AF.Sqrt), reads=[pn], writes=[t4])
                S.op(V_, lambda e: e.tensor_scalar_max(out=t4[:], in0=t4[:], scalar1=1e-12), reads=[t4], writes=[t4])
                S.op(V_, lambda e: e.reciprocal(out=t4[:], in_=t4[:]), reads=[t4], writes=[t4])
                S.op(V_, lambda e: e.tensor_tensor(out=kk[:], in0=kk[:], in1=t4[:], op=ALU.mult), reads=[kk, t4], writes=[kk])
                S.op(V_, lambda e: e.tensor_tensor(out=kmod[:], in0=aT[:], in1=rv[:, 3, :].unsqueeze(2).to_broadcast([128, 4, 128]), op=ALU.mult),
                     reads=[aT, rv], writes=[kmod])
                S.op(V_, lambda e: e.tensor_tensor(out=kmod[:], in0=kmod[:], in1=omka[:, 0:4].unsqueeze(2).to_broadcast([128, 4, 128]), op=ALU.add),
                     reads=[kmod, omka], writes=[kmod])
                S.op(V_, lambda e: e.tensor_tensor(out=kmod[:], in0=kmod[:], in1=pm[:, 4:8, :], op=ALU.mult), reads=[kmod, pm], writes=[kmod])

            def frontB(b):
                ag, rg, Vt, Vtb, bgT, kgT, gC, gt, bsum, Aak, Arb, Ark = ifc1[b % 2]
                pg = nps()
                S.op(P_, lambda e, pg=pg: e.matmul(pg[:, :], lhsT=sg[:], rhs=gup[:], start=True, stop=True), reads=[sg, gup], writes=[pg])
                S.op(V_, lambda e, pg=pg: e.tensor_copy(out=gt[:], in_=pg[:, :]), reads=[pg], writes=[gt])
                S.op(G_, lambda e: e.tensor_copy(out=gC[:], in_=E1[:].rearrange('p c (h j) -> p c h j', j=64)[:, :, :, 63]), reads=[E1], writes=[gC])
                S.op(V_, lambda e: e.scalar_tensor_tensor(out=ag[:], in0=kk[:], scalar=-1.0, in1=E2[:], op0=ALU.mult, op1=ALU.mult),
                     reads=[kk, E2], writes=[ag])
                S.op(V_, lambda e: e.tensor_tensor(out=bg[:], in0=kk[:], in1=aT[:], op=ALU.mult), reads=[kk, aT], writes=[bg])
                S.op(V_, lambda e: e.tensor_tensor(out=bg[:], in0=bg[:], in1=E3[:], op=ALU.mult), reads=[bg, E3], writes=[bg])
                S.op(G_, lambda e: e.tensor_tensor(out=kg[:], in0=kmod[:], in1=E3[:], op=ALU.mult), reads=[kmod, E3], writes=[kg])
                S.op(V_, lambda e: e.tensor_tensor(out=rg[:], in0=pm[:, 0:4, :], in1=E1[:], op=ALU.mult), reads=[pm, E1], writes=[rg])
                S.op(G_, lambda e: e.tensor_tensor(out=rk[:], in0=pm[:, 0:4, :], in1=kmod[:], op=ALU.mult), reads=[pm, kmod], writes=[rk])
                S.op(G_, lambda e: e.tensor_tensor(out=rk[:], in0=rk[:], in1=rv[:, 4, :].unsqueeze(2).to_broadcast([128, 4, 128]), op=ALU.mult),
                     reads=[rk, rv], writes=[rk])
                transpose_to(Vt, lambda n: Vt[:].rearrange('p (n j) -> p n j', j=128), pm, [pm[:, 8 + i, :] for i in range(4)], ident, F32, A_)
                S.op(A_, lambda e: e.copy(out=Vtb[:], in_=Vt[:]), reads=[Vt], writes=[Vtb])
                transpose_to(bgT, lambda n: bgT[:].rearrange('p (n j) -> p n j', j=128), bg, [bg[:, i, :] for i in range(4)], identb, BF16, V_)
                transpose_to(kgT, lambda n: kgT[:].rearrange('p (n j) -> p n j', j=128), kg, [kg[:, i, :] for i in range(4)], identb, BF16, A_)
                pbn = nps()
                for c in range(4):
                    S.op(P_, lambda e, c=c, pbn=pbn: e.matmul(pbn[:, 2 * c:2 * c + 2], lhsT=rk[:, c, :], rhs=hsel[:], start=True, stop=True),
                         reads=[rk, hsel], writes=[pbn], inc=(c == 3))
                S.op(V_, lambda e, pbn=pbn: e.tensor_copy(out=bsum[:], in_=pbn[:, 0:8]), reads=[pbn], writes=[bsum])

            def frontC(b):
                ag, rg, Vt, Vtb, bgT, kgT, gC, gt, bsum, Aak, Arb, Ark = ifc1[b % 2]
                import os
                _nd = int(os.environ.get('P1_ND', '5')); _nomask = os.environ.get('P1_NOMASK') == '1'; _nomm = os.environ.get('P1_NOMM') == '1'
                for (dst, lt, rt, mask) in ((Nm, bg, ag, MUs), (NTm, ag, bg, MLs), (Aak, kg, ag, MUs), (Arb, bg, rg, MU), (Ark, kg, rg, MU))[:_nd]:
                    for par in range(2):
                        pb = nps()
                        for hh in range(4):
                            h = 2 * hh + par
                            S.op(P_, lambda e, h=h, hh=hh, pb=pb, lt=lt, rt=rt: e.matmul(
                                pb[:, hh * 128:(hh + 1) * 128], lhsT=lt[hrows(h), h // 2, :], rhs=rt[hrows(h), h // 2, :], start=True, stop=True),
                                reads=[lt, rt], writes=[pb], inc=(hh == 3))
                        S.op(V_, lambda e, pb=pb, dst=dst, par=par, mask=mask: e.tensor_tensor(
                            out=dst[:, par:8:2, :], in0=pb[:, :].rearrange('p (n j) -> p n j', j=128),
                            in1=mask[:].unsqueeze(1).to_broadcast([128, 4, 128]), op=ALU.mult), reads=[pb, mask], writes=[dst])

            def back(b):
                ag, rg, Vt, Vtb, bgT, kgT, gC, gt, bsum, Aak, Arb, Ark = ifc1[b % 2]
                S.op(V_, lambda e: e.tensor_tensor(out=Rm1[:], in0=Nm[:], in1=ident[:].unsqueeze(1).to_broadcast([128, 8, 128]), op=ALU.add),
                     reads=[Nm, ident], writes=[Rm1])
                Pc, PTc, Rc = Nm, NTm, Rm1
                for lvl in range(5):
                    Pn, PTn, Rn = Pm[lvl % 2], PTm[lvl % 2], Rm1
                    last = (lvl == 4)
                    for hg in range(2):
                        hs = slice(hg * 4, hg * 4 + 4)
                        pbT = nps()
                        for hh in range(4):
                            h = hg * 4 + hh
                            S.op(P_, lambda e, h=h, hh=hh, pbT=pbT, Pc=Pc, PTc=PTc: e.matmul(
                                pbT[:, hh * 128:(hh + 1) * 128], lhsT=Pc[:, h, :], rhs=PTc[:, h, :], start=True, stop=True),
                                reads=[Pc, PTc], writes=[pbT], inc=(hh == 3))
                        S.op(A_, lambda e, pbT=pbT, PTn=PTn, hs=hs: e.copy(out=PTn[:, hs, :], in_=pbT[:, :].rearrange('p (n j) -> p n j', j=128)),
                             reads=[pbT], writes=[PTn])
                        if not last:
                            pbP = nps()
                            for hh in range(4):
                                h = hg * 4 + hh
                                S.op(P_, lambda e, h=h, hh=hh, pbP=pbP, Pc=Pc, PTc=PTc: e.matmul(
                                    pbP[:, hh * 128:(hh + 1) * 128], lhsT=PTc[:, h, :], rhs=Pc[:, h, :], start=True, stop=True),
                                    reads=[Pc, PTc], writes=[pbP], inc=(hh == 3))
                            S.op(V_, lambda e, pbP=pbP, Pn=Pn, hs=hs: e.tensor_copy(out=Pn[:, hs, :], in_=pbP[:, :].rearrange('p (n j) -> p n j', j=128)),
                                 reads=[pbP], writes=[Pn])
                        pbR = nps()
                        for hh in range(4):
                            h = hg * 4 + hh
                            S.op(P_, lambda e, h=h, hh=hh, pbR=pbR, PTn=PTn, Rc=Rc: e.matmul(
                                pbR[:, hh * 128:(hh + 1) * 128], lhsT=PTn[:, h, :], rhs=Rc[:, h, :], start=True, stop=True),
                                reads=[PTn, Rc], writes=[pbR], inc=(hh == 3))
                        S.op(V_, lambda e, pbR=pbR, Rn=Rn, Rc=Rc, hs=hs: e.tensor_tensor(
                            out=Rn[:, hs, :], in0=pbR[:, :].rearrange('p (n j) -> p n j', j=128), in1=Rc[:, hs, :], op=ALU.add),
                            reads=[pbR, Rc], writes=[Rn])
                    Pc, PTc, Rc = Pn, PTn, Rn
                Rf = Rc
                pY = nps()
                for cc in range(2):
                    cr = slice(cc * 64, cc * 64 + 64)
                    pX = nps()
                    for p in range(4):
                        for h in (2 * p, 2 * p + 1):
                            hc = slice(h * 64, h * 64 + 64)
                            S.op(P_, lambda e, p=p, h=h, hc=hc, cr=cr, pX=pX: e.matmul(pX[cr, hc], lhsT=ag[:, p, cr], rhs=STb[:, p, (h % 2) * 64:(h % 2) * 64 + 64],
                                                                                       start=True, stop=False), reads=[ag, STb], writes=[pX], inc=False)
                            S.op(P_, lambda e, h=h, hc=hc, cr=cr, pX=pX: e.matmul(pX[cr, hc], lhsT=Aak[cr, h, cr], rhs=Vtb[cr, hc],
                                                                                  start=False, stop=True), reads=[Aak, Vtb], writes=[pX], inc=(h == 7))
                    S.op(A_, lambda e, cr=cr, pX=pX: e.copy(out=xs[cr, :, :], in_=pX[cr, :].rearrange('p (h j) -> p h j', j=64)), reads=[pX], writes=[xs])
                    pU = nps()
                    for h in range(8):
                        hc = slice(h * 64, h * 64 + 64)
                        S.op(P_, lambda e, h=h, hc=hc, cr=cr, pU=pU: e.matmul(pU[cr, hc], lhsT=Rf[cr, h, cr], rhs=xs[cr, h, :], start=True, stop=True),
                             reads=[Rf, xs], writes=[pU], inc=(h == 7))
                    S.op(V_, lambda e, cr=cr, pU=pU: e.tensor_copy(out=us[cr, :, :], in_=pU[cr, :].rearrange('p (h j) -> p h j', j=64)), reads=[pU], writes=[us])
                    for p in range(4):
                        for h in (2 * p, 2 * p + 1):
                            hc = slice(h * 64, h * 64 + 64)
                            S.op(P_, lambda e, p=p, h=h, hc=hc, cr=cr: e.matmul(pY[cr, hc], lhsT=rg[:, p, cr], rhs=STb[:, p, (h % 2) * 64:(h % 2) * 64 + 64],
                                                                                start=True, stop=False), reads=[rg, STb], writes=[pY], inc=False)
                            S.op(P_, lambda e, h=h, hc=hc, cr=cr: e.matmul(pY[cr, hc], lhsT=Arb[cr, h, cr], rhs=us[cr, h, :], start=False, stop=False),
                                 reads=[Arb, us], writes=[pY], inc=False)
                            S.op(P_, lambda e, h=h, hc=hc, cr=cr: e.matmul(pY[cr, hc], lhsT=Ark[cr, h, cr], rhs=Vtb[cr, hc], start=False, stop=True),
                                 reads=[Ark, Vtb], writes=[pY], inc=(h == 7))
                    pS = nps()
                    for h in range(8):
                        hc = slice(h * 64, h * 64 + 64)
                        oc = slice((h // 2) * 64, (h // 2) * 64 + 64)
                        S.op(P_, lambda e, h=h, hc=hc, cr=cr, oc=oc, pS=pS: e.matmul(pS[hrows(h), oc], lhsT=bgT[cr, hc], rhs=us[cr, h, :], start=True, stop=False),
                             reads=[bgT, us], writes=[pS], inc=False)
                        S.op(P_, lambda e, h=h, hc=hc, cr=cr, oc=oc, pS=pS: e.matmul(pS[hrows(h), oc], lhsT=kgT[cr, hc], rhs=Vtb[cr, hc], start=False, stop=True),
                             reads=[kgT, Vtb], writes=[pS], inc=(h == 7))
                    S.op(V_, lambda e, pS=pS: e.tensor_tensor(out=ST[:], in0=pS[:, 0:256].rearrange('p (n j) -> p n j', j=64), in1=ST[:], op=ALU.add),
                         reads=[pS, ST], writes=[ST])
                    S.op(V_, lambda e, cc=cc: e.tensor_tensor(out=ST[:], in0=ST[:], in1=gC[:, :, cc:cc + 1].to_broadcast([128, 4, 64]), op=ALU.mult),
                         reads=[ST, gC], writes=[ST])
                    S.op(V_, lambda e: e.tensor_tensor(out=STb[:].rearrange('p c (t v) -> p c t v', t=2),
                                                       in0=ST[:].unsqueeze(2).to_broadcast([128, 4, 2, 64]),
                                                       in1=hsel[:].unsqueeze(1).unsqueeze(3).to_broadcast([128, 4, 2, 64]), op=ALU.mult),
                         reads=[ST, hsel], writes=[STb])
                S.op(A_, lambda e: e.copy(out=ysb[:], in_=pY[:, :].rearrange('p (h j) -> p h j', j=64)), reads=[pY], writes=[ysb])
                S.op(V_, lambda e: e.tensor_reduce(out=st8[:, 0, :], in_=ysb[:], axis=AX.X, op=ALU.add), reads=[ysb], writes=[st8])
                S.op(V_, lambda e: e.tensor_scalar(out=st8[:, 0, :], in0=st8[:, 0, :], scalar1=1.0 / 64, scalar2=None, op0=ALU.mult), reads=[st8], writes=[st8])
                S.op(V_, lambda e: e.tensor_tensor(out=yc[:], in0=ysb[:], in1=st8[:, 0, :].unsqueeze(2).to_broadcast([128, 8, 64]), op=ALU.subtract),
                     reads=[ysb, st8], writes=[yc])
                S.op(V_, lambda e: e.tensor_tensor(out=ysb[:], in0=yc[:], in1=yc[:], op=ALU.mult), reads=[yc], writes=[ysb])
                S.op(V_, lambda e: e.tensor_reduce(out=st8[:, 1, :], in_=ysb[:], axis=AX.X, op=ALU.add), reads=[ysb], writes=[st8])
                S.op(V_, lambda e: e.tensor_scalar(out=st8[:, 1, :], in0=st8[:, 1, :], scalar1=1.0 / 64, scalar2=GN_EPS, op0=ALU.mult, op1=ALU.add),
                     reads=[st8], writes=[st8])
                S.op(A_, lambda e: e.activation(out=st8[:, 1, :], in_=st8[:, 1, :], func=AF.Sqrt), reads=[st8], writes=[st8])
                S.op(V_, lambda e: e.reciprocal(out=st8[:, 1, :], in_=st8[:, 1, :]), reads=[st8], writes=[st8])
                S.op(V_, lambda e: e.tensor_tensor(out=yc[:], in0=yc[:], in1=st8[:, 1, :].unsqueeze(2).to_broadcast([128, 8, 64]), op=ALU.mult),
                     reads=[yc, st8], writes=[yc])
                ycf = yc[:].rearrange('p h j -> p (h j)')
                S.op(V_, lambda e: e.tensor_tensor(out=ycf, in0=ycf, in1=lnw[:], op=ALU.mult), reads=[yc, lnw], writes=[yc])
                S.op(V_, lambda e: e.tensor_tensor(out=ycf, in0=ycf, in1=lnb[:], op=ALU.add), reads=[yc, lnb], writes=[yc])
                S.op(V_, lambda e: e.tensor_tensor(out=ysb[:], in0=Vt[:].rearrange('p (h j) -> p h j', j=64),
                                                   in1=bsum[:, 0:8].unsqueeze(2).to_broadcast([128, 8, 64]), op=ALU.mult), reads=[Vt, bsum], writes=[ysb])
                S.op(V_, lambda e: e.tensor_tensor(out=yc[:], in0=yc[:], in1=ysb[:], op=ALU.add), reads=[yc, ysb], writes=[yc])
                S.op(V_, lambda e: e.tensor_tensor(out=ycf, in0=ycf, in1=gt[:], op=ALU.mult), reads=[yc, gt], writes=[yc])
                outproj_add(b, yc, ycf, wo1, True, None, ybf, yT)

            if do_p1:
                frontA(0)
                frontB(0)
                frontC(0)
                for b in range(NB):
                    _la = S.record(lambda: (frontA(b + 1), frontB(b + 1), frontC(b + 1))) if b + 1 < NB else []
                    _lb = S.record(lambda: back(b), pspool=(4, 8))
                    S.run_interleaved(_la, _lb)


        S.barrier()
        AR.reset()
        if True:
            sb = AR.alloc
            wh_in = sb('wh_in', [128, 8, 2048], BF16)
            wo2 = sb('wo2', [128, 4, D], BF16)
            S.dma(G_, lambda e: e.dma_start(out=wh_in[:], in_=win_d[:, 1792:3840].rearrange('(k p) n -> p k n', p=128)), writes=[wh_in])
            S.dma(G_, lambda e: e.dma_start(out=wo2[:], in_=wout_d[512:1024, :].rearrange('(k p) n -> p k n', p=128)), writes=[wo2])
            cw = sb('cw', [128, 4, 12]); lbl = sb('lbl', [128, 2, 4]); lb = sb('lb', [128, 4]); oml = sb('oml', [128, 4])
            hng = sb('hng', [128, 512])
            ld(cw, cw_d); ld(lbl, lbl_d); ld(hng, hng_d.partition_broadcast(128))
            S.op(V_, lambda e: e.tensor_tensor(out=lb[:], in0=lbl[:, 0, :], in1=lbl[:, 1, :], op=ALU.subtract), reads=[lbl], writes=[lb])
            S.op(A_, lambda e: e.activation(out=lb[:], in_=lb[:], func=AF.Sigmoid), reads=[lb], writes=[lb])
            S.op(V_, lambda e: e.tensor_scalar(out=oml[:], in0=lb[:], scalar1=-1.0, scalar2=1.0, op0=ALU.mult, op1=ALU.add), reads=[lb], writes=[oml])
            xts = [sb('xt0', [128, D]), sb('xt1', [128, D])]
            nb_t = sb('nb', [128, D], BF16); nT = sb('nT', [128, 8, 128], BF16)
            sst = sb('sst', [128, 2])
            ph1 = sb('ph', [128, 12, 131])
            cv = sb('cv', [128, 12, 128]); tmpcs = [sb('tc0', [128, 12, 128]), sb('tc1', [128, 12, 128])]
            cvi = Tk('cvi', cv.t); cvf = Tk('cvf', cv.t)
            phg = {0: ph1, 4: Tk('phf', ph1.t), 8: Tk('phi', ph1.t)}
            cvg = {0: cv, 4: cvf, 8: cvi}
            tmg = {0: tmpcs[0], 4: Tk('tcf', tmpcs[0].t), 8: tmpcs[1]}
            qs = sb('qs', [128, 4, 128]); fg = sb('fg', [128, 4, 128]); lf = sb('lf', [128, 4, 128]); bcum = sb('bcum', [128, 4, 128])
            eb = sb('eb', [128, 4, 128]); enb = sb('enb', [128, 4, 128]); kt = sb('kt', [128, 4, 128])
            SH = sb('SH', [128, 4, 128])
            ifc = [(sb('qt', [128, 4, 128]), sb('eC', [128, 4, 2]), sb('vI', [128, 512]), sb('ktT', [128, 512]), sb('gs', [128, 512]), sb('sc', [128, 4, 128]))
                   for _ in range(2)]
            osb = sb('osb', [128, 4, 128]); osq = sb('osq', [128, 4, 128]); st4 = sb('st4', [128, 4])
            ybf = sb('ybf', [128, 512], BF16); yT = sb('yT', [128, 4, 128], BF16)
            S.op(V_, lambda e: e.memset(SH[:], 0.0), writes=[SH])
            S.op(V_, lambda e: e.memset(ph1[:, :, 0:3], 0.0), writes=[phg[0], phg[4], phg[8]])

            def front2(b):
                qt, eC, vI, ktT, gs, sc = ifc[b % 2]
                xt = load_norm_transpose(b, xts, nb_t, nT, sst, g1b)
                cur = ph1
                for g0 in (4, 0, 8):
                    pb = nps()
                    for i in range(4):
                        for k in range(8):
                            S.op(P_, lambda e, i=i, k=k, g0=g0, pb=pb: e.matmul(
                                pb[:, i * 128:(i + 1) * 128], lhsT=wh_in[:, k, (g0 + i) * 128:(g0 + i + 1) * 128], rhs=nT[:, k, :],
                                start=(k == 0), stop=(k == 7)), reads=[wh_in, nT], writes=[pb], inc=(k == 7 and i == 3))
                    src = pb[:, :].rearrange('p (n j) -> p n j', j=128)
                    if g0 != 0:
                        S.op(A_, lambda e, src=src, g0=g0, cur=cur: e.copy(out=cur[:, g0:g0 + 4, 3:131], in_=src), reads=[pb], writes=[phg[g0]])
                    else:
                        S.op(V_, lambda e, src=src, g0=g0, cur=cur: e.tensor_copy(out=cur[:, g0:g0 + 4, 3:131], in_=src), reads=[pb], writes=[phg[g0]])
                pgt = nps()
                for k in range(8):
                    S.op(P_, lambda e, k=k, pgt=pgt: e.matmul(pgt[:, :], lhsT=nT[:, k, :], rhs=wh_in[:, k, 1536:2048], start=(k == 0), stop=(k == 7)),
                         reads=[nT, wh_in], writes=[pgt], inc=(k == 7))
                S.op(A_, lambda e, pgt=pgt: e.activation(out=gs[:], in_=pgt[:, :], func=AF.Silu), reads=[pgt], writes=[gs])
                for (eng, c0) in ((V_, 4), (G_, 8), (V_, 0)):
                    c1 = c0 + 4
                    phx, cvx, tm = phg[c0], cvg[c0], tmg[c0]
                    for w in range(4):
                        cwb = cw[:, w, c0:c1].unsqueeze(2).to_broadcast([128, 4, 128])
                        if w == 0:
                            S.op(eng, lambda e, cur=cur, cwb=cwb, c0=c0, c1=c1: e.tensor_tensor(out=cv[:, c0:c1, :], in0=cur[:, c0:c1, 0:128], in1=cwb, op=ALU.mult),
                                 reads=[phx, cw], writes=[cvx])
                        else:
                            S.op(eng, lambda e, cur=cur, cwb=cwb, w=w, tm=tm, c0=c0, c1=c1: e.tensor_tensor(out=tm[:, c0:c1, :], in0=cur[:, c0:c1, w:w + 128], in1=cwb, op=ALU.mult),
                                 reads=[phx, cw], writes=[tm])
                            S.op(eng, lambda e, tm=tm, c0=c0, c1=c1: e.tensor_tensor(out=cv[:, c0:c1, :], in0=cv[:, c0:c1, :], in1=tm[:, c0:c1, :], op=ALU.add),
                                 reads=[cvx, tm], writes=[cvx])
                S.op(G_, lambda e, cur=cur: e.tensor_copy(out=cur[:, :, 0:3], in_=cur[:, :, 128:131]), reads=[phg[0], phg[4], phg[8]], writes=[phg[0], phg[4], phg[8]])
                S.op(A_, lambda e: e.activation(out=fg[:], in_=cv[:, 4:8, :], func=AF.Sigmoid), reads=[cvf], writes=[fg])
                S.op(V_, lambda e: e.tensor_tensor(out=fg[:], in0=fg[:], in1=oml[:, 0:4].unsqueeze(2).to_broadcast([128, 4, 128]), op=ALU.mult),
                     reads=[fg, oml], writes=[fg])
                S.op(V_, lambda e: e.tensor_tensor(out=fg[:], in0=fg[:], in1=lb[:, 0:4].unsqueeze(2).to_broadcast([128, 4, 128]), op=ALU.add),
                     reads=[fg, lb], writes=[fg])
                S.op(A_, lambda e: e.activation(out=lf[:], in_=fg[:], func=AF.Ln), reads=[fg], writes=[lf])
                S.op(A_, lambda e: e.activation(out=qs[:], in_=cv[:, 0:4, :], func=AF.Silu), reads=[cv], writes=[qs])
                for c in range(4):
                    S.op(V_, lambda e, c=c: e.tensor_tensor_scan(out=bcum[:, c, :], data0=smask[:], data1=lf[:, c, :], initial=0.0, op0=ALU.mult, op1=ALU.add),
                         reads=[smask, lf], writes=[bcum])
                S.op(A_, lambda e: e.activation(out=eb[:], in_=bcum[:], func=AF.Exp), reads=[bcum], writes=[eb])
                S.op(A_, lambda e: e.activation(out=enb[:], in_=bcum[:], func=AF.Exp, scale=-1.0), reads=[bcum], writes=[enb])
                S.op(G_, lambda e: e.tensor_copy(out=eC[:], in_=eb[:].rearrange('p c (h j) -> p c h j', j=64)[:, :, :, 63]), reads=[eb], writes=[eC])
                S.op(V_, lambda e: e.tensor_tensor(out=qt[:], in0=qs[:], in1=eb[:], op=ALU.mult), reads=[qs, eb], writes=[qt])
                S.op(V_, lambda e: e.tensor_scalar(out=kt[:], in0=fg[:], scalar1=-1.0, scalar2=1.0, op0=ALU.mult, op1=ALU.add), reads=[fg], writes=[kt])
                S.op(V_, lambda e: e.tensor_tensor(out=kt[:], in0=kt[:], in1=enb[:], op=ALU.mult), reads=[kt, enb], writes=[kt])
                transpose_to(vI, lambda n: vI[:].rearrange('p (n j) -> p n j', j=128), cvi, [cv[:, 8 + i, :] for i in range(4)], ident, F32, A_)
                transpose_to(ktT, lambda n: ktT[:].rearrange('p (n j) -> p n j', j=128), kt, [kt[:, i, :] for i in range(4)], ident, F32, V_)
                psc = nps()
                for h in range(4):
                    S.op(P_, lambda e, h=h, psc=psc: e.matmul(psc[:, h * 128:(h + 1) * 128], lhsT=kt[:, h, :], rhs=qt[:, h, :], start=True, stop=True),
                         reads=[kt, qt], writes=[psc], inc=(h == 3))
                S.op(V_, lambda e, psc=psc: e.tensor_tensor(out=sc[:], in0=psc[:, :].rearrange('p (n j) -> p n j', j=128),
                                                            in1=MU[:].unsqueeze(1).to_broadcast([128, 4, 128]), op=ALU.mult), reads=[psc, MU], writes=[sc])

            def back2(b):
                qt, eC, vI, ktT, gs, sc = ifc[b % 2]
                pO = nps()
                for cc in range(2):
                    cr = slice(cc * 64, cc * 64 + 64)
                    for h in range(4):
                        hc = slice(h * 128, h * 128 + 128)
                        S.op(P_, lambda e, h=h, hc=hc, cr=cr: e.matmul(pO[cr, hc], lhsT=sc[cr, h, cr], rhs=vI[cr, hc], start=True, stop=False),
                             reads=[sc, vI], writes=[pO], inc=False)
                        S.op(P_, lambda e, h=h, hc=hc, cr=cr: e.matmul(pO[cr, hc], lhsT=qt[:, h, cr], rhs=SH[:, h, :], start=False, stop=True),
                             reads=[qt, SH], writes=[pO], inc=(h == 3))
                    pS = nps()
                    for h in range(4):
                        hc = slice(h * 128, h * 128 + 128)
                        S.op(P_, lambda e, h=h, hc=hc, cr=cr, pS=pS: e.matmul(pS[:, hc], lhsT=ktT[cr, hc], rhs=vI[cr, hc], start=True, stop=True),
                             reads=[ktT, vI], writes=[pS], inc=(h == 3))
                    S.op(V_, lambda e, pS=pS: e.tensor_tensor(out=SH[:], in0=pS[:, :].rearrange('p (n j) -> p n j', j=128), in1=SH[:], op=ALU.add),
                         reads=[pS, SH], writes=[SH])
                    S.op(V_, lambda e, cc=cc: e.tensor_tensor(out=SH[:], in0=SH[:], in1=eC[:, :, cc:cc + 1].to_broadcast([128, 4, 128]), op=ALU.mult),
                         reads=[SH, eC], writes=[SH])
                S.op(A_, lambda e: e.copy(out=osb[:], in_=pO[:, :].rearrange('p (h j) -> p h j', j=128)), reads=[pO], writes=[osb])
                S.op(V_, lambda e: e.tensor_tensor(out=osq[:], in0=osb[:], in1=osb[:], op=ALU.mult), reads=[osb], writes=[osq])
                S.op(V_, lambda e: e.tensor_reduce(out=st4[:], in_=osq[:], axis=AX.X, op=ALU.add), reads=[osq], writes=[st4])
                S.op(V_, lambda e: e.tensor_scalar(out=st4[:], in0=st4[:], scalar1=1.0 / 128, scalar2=RMS_EPS, op0=ALU.mult, op1=ALU.add), reads=[st4], writes=[st4])
                S.op(A_, lambda e: e.activation(out=st4[:], in_=st4[:], func=AF.Sqrt), reads=[st4], writes=[st4])
                S.op(V_, lambda e: e.reciprocal(out=st4[:], in_=st4[:]), reads=[st4], writes=[st4])
                S.op(V_, lambda e: e.tensor_tensor(out=osb[:], in0=osb[:], in1=st4[:, 0:4].unsqueeze(2).to_broadcast([128, 4, 128]), op=ALU.mult),
                     reads=[osb, st4], writes=[osb])
                osf = osb[:].rearrange('p h j -> p (h j)')
                S.op(V_, lambda e: e.tensor_tensor(out=osf, in0=osf, in1=hng[:], op=ALU.mult), reads=[osb, hng], writes=[osb])
                S.op(V_, lambda e: e.tensor_tensor(out=osf, in0=osf, in1=gs[:], op=ALU.mult), reads=[osb, gs], writes=[osb])
                outproj_add(b, osb, osf, wo2, False, None, ybf, yT)

            if do_p2:
                front2(0)
                for b in range(NB):
                    _la = S.record(lambda: front2(b + 1)) if b + 1 < NB else []
                    _lb = S.record(lambda: back2(b), pspool=(4, 8))
                    S.run_interleaved(_la, _lb)

        S.barrier()
        AR.reset()
        if True:
            sb = AR.alloc
            TBK = min(512, T)
            NTB = T // TBK
            NBt = TBK // 128
            n2T = sb('n2T', [128, 8, T], BF16)
            wr = sb('wr', [128, 8, 36]); brb = sb('brb', [128, 36])
            S.dma('sync', lambda e: e.dma_start(out=wr[:], in_=wr_d.rearrange('(k p) n -> p k n', p=128)), writes=[wr])
            ld(brb, br_d.partition_broadcast(128))
            wgb = [sb('wg0', [128, 8, DE], BF16), sb('wg1', [128, 8, DE], BF16)]
            wub = [sb('wu0', [128, 8, DE], BF16), sb('wu1', [128, 8, DE], BF16)]
            wdb = [sb('wd0', [128, 4, D], BF16), sb('wd1', [128, 4, D], BF16)]

            def load_expert(e_):
                i = e_ % 2
                S.dma(G_, lambda e: e.dma_start(out=wgb[i][:], in_=wg_d[e_].rearrange('(k p) n -> p k n', p=128)), writes=[wgb[i]])
                S.dma(G_, lambda e: e.dma_start(out=wub[i][:], in_=wu_d[e_].rearrange('(k p) n -> p k n', p=128)), writes=[wub[i]])
                S.dma(G_, lambda e: e.dma_start(out=wdb[i][:], in_=wd_d[e_].rearrange('(k p) n -> p k n', p=128)), writes=[wdb[i]])
            if n_exp > 0:
                load_expert(0)
            if n_exp > 1:
                load_expert(1)
            g2b = sb('g2b', [128, D]); gfb = sb('gfb', [128, D])
            ld(g2b, g2_d.partition_broadcast(128)); ld(gfb, gf_d.partition_broadcast(128))
            n2s = [sb('n2a', [128, D]), sb('n2b', [128, D])]; n2T32s = [sb('n2T32a', [128, 8, 128]), sb('n2T32b', [128, 8, 128])]
            ssts = [sb('ssta', [128, 2]), sb('sstb', [128, 2])]
            n2, n2T32, sst = n2s[0], n2T32s[0], ssts[0]
            lgs = [sb(f'lg{t}', [128, NBt, 36]) for t in range(NTB)]
            gts = [sb(f'gates{t}', [128, NBt, 32]) for t in range(NTB)]
            mg = sb('mg', [128, NBt, 4]); gmx = sb('gmx', [128, NBt]); gex = sb('gex', [128, NBt, 4]); gw = sb('gw', [128, NBt])
            le = sb('le', [128, NBt, 32]); el = sb('el', [128, NBt, 8]); m1 = sb('m1', [128, NBt]); k1 = sb('k1', [128, NBt, 8])
            el2 = sb('el2', [128, NBt, 8]); m2 = sb('m2', [128, NBt]); k2 = sb('k2', [128, NBt, 8]); p2 = sb('p2', [128, NBt]); w1 = sb('w1', [128, NBt])
            ge = sb('ge', [128, NBt, 8])
            hT = sb('hT', [128, 4, T], BF16)
            sgs = [sb('sg0', [128, TBK]), sb('sg1', [128, TBK])]
            ot = [(n2s[0], n2s[0][:]), (n2s[1], n2s[1][:])]

            def front(tb):
                lg = lgs[tb]
                for j in range(NBt):
                    b = tb * NBt + j
                    n2, n2T32, sst = n2s[b % 2], n2T32s[b % 2], ssts[b % 2]
                    norm_block((hb[b], hb[b][:]), g2b, (n2, n2[:]), sst)
                    for half in range(2):
                        pbx = nps()
                        for i in range(4):
                            S.op(P_, lambda e, i=i, pbx=pbx, half=half, n2=n2: e.transpose(pbx[:, i * 128:(i + 1) * 128], n2[:, (half * 4 + i) * 128:(half * 4 + i + 1) * 128], ident[:]),
                                 reads=[n2, ident], writes=[pbx], inc=(i == 3))
                        srcx = pbx[:, :].rearrange('p (n j) -> p n j', j=128)
                        S.op(A_, lambda e, srcx=srcx, half=half, n2T32=n2T32: e.copy(out=n2T32[:, half * 4:half * 4 + 4, :], in_=srcx), reads=[pbx], writes=[n2T32])
                        S.op(V_, lambda e, half=half, b=b, n2T32=n2T32: e.tensor_copy(out=n2T[:, half * 4:half * 4 + 4, b * 128:(b + 1) * 128], in_=n2T32[:, half * 4:half * 4 + 4, :]),
                             reads=[n2T32], writes=[n2T])
                    pr = nps()
                    for k in range(8):
                        S.op(P_, lambda e, k=k, pr=pr: e.matmul(pr[:, 0:36], lhsT=n2T32[:, k, :], rhs=wr[:, k, :], start=(k == 0), stop=(k == 7)),
                             reads=[n2T32, wr], writes=[pr], inc=(k == 7))
                    S.op(V_, lambda e, j=j, pr=pr: e.tensor_tensor(out=lg[:, j, :], in0=pr[:, 0:36], in1=brb[:], op=ALU.add), reads=[pr, brb], writes=[lg])

            def gating(tb):
                lg = lgs[tb]; gates = gts[tb]
                NB_ = NBt
                lgg = lg[:, :, 0:4]
                S.op(V_, lambda e: e.tensor_reduce(out=gmx[:], in_=lgg, axis=AX.X, op=ALU.max), reads=[lg], writes=[gmx])
                S.op(V_, lambda e: e.tensor_tensor(out=mg[:], in0=lgg, in1=gmx[:, 0:NB_].unsqueeze(2).to_broadcast([128, NB_, 4]), op=ALU.is_equal),
                     reads=[lg, gmx], writes=[mg])
                S.op(V_, lambda e: e.tensor_tensor(out=gex[:], in0=lgg, in1=gmx[:, 0:NB_].unsqueeze(2).to_broadcast([128, NB_, 4]), op=ALU.subtract),
                     reads=[lg, gmx], writes=[gex])
                S.op(A_, lambda e: e.activation(out=gex[:], in_=gex[:], func=AF.Exp), reads=[gex], writes=[gex])
                S.op(V_, lambda e: e.tensor_reduce(out=gw[:], in_=gex[:], axis=AX.X, op=ALU.add), reads=[gex], writes=[gw])
                S.op(V_, lambda e: e.reciprocal(out=gw[:], in_=gw[:]), reads=[gw], writes=[gw])
                S.op(V_, lambda e: e.tensor_tensor(out=le[:].rearrange('p b (g j) -> p b g j', j=8), in0=lg[:, :, 4:36].rearrange('p b (g j) -> p b g j', j=8),
                                                   in1=mg[:].unsqueeze(3).to_broadcast([128, NB_, 4, 8]), op=ALU.mult), reads=[lg, mg], writes=[le])
                S.op(V_, lambda e: e.tensor_reduce(out=el[:], in_=le[:].rearrange('p b (g j) -> p b j g', j=8), axis=AX.X, op=ALU.add), reads=[le], writes=[el])
                S.op(V_, lambda e: e.tensor_reduce(out=m1[:], in_=el[:], axis=AX.X, op=ALU.max), reads=[el], writes=[m1])
                S.op(V_, lambda e: e.tensor_tensor(out=k1[:], in0=el[:], in1=m1[:, 0:NB_].unsqueeze(2).to_broadcast([128, NB_, 8]), op=ALU.is_equal),
                     reads=[el, m1], writes=[k1])
                S.op(V_, lambda e: e.scalar_tensor_tensor(out=el2[:], in0=k1[:], scalar=-1e30, in1=el[:], op0=ALU.mult, op1=ALU.add), reads=[k1, el], writes=[el2])
                S.op(V_, lambda e: e.tensor_reduce(out=m2[:], in_=el2[:], axis=AX.X, op=ALU.max), reads=[el2], writes=[m2])
                S.op(V_, lambda e: e.tensor_tensor(out=k2[:], in0=el2[:], in1=m2[:, 0:NB_].unsqueeze(2).to_broadcast([128, NB_, 8]), op=ALU.is_equal),
                     reads=[el2, m2], writes=[k2])
                S.op(V_, lambda e: e.tensor_tensor(out=p2[:], in0=m2[:], in1=m1[:], op=ALU.subtract), reads=[m1, m2], writes=[p2])
                S.op(A_, lambda e: e.activation(out=p2[:], in_=p2[:], func=AF.Exp), reads=[p2], writes=[p2])
                S.op(V_, lambda e: e.tensor_scalar(out=w1[:], in0=p2[:], scalar1=1.0, scalar2=None, op0=ALU.add), reads=[p2], writes=[w1])
                S.op(V_, lambda e: e.reciprocal(out=w1[:], in_=w1[:]), reads=[w1], writes=[w1])
                S.op(V_, lambda e: e.tensor_tensor(out=p2[:], in0=p2[:], in1=w1[:], op=ALU.mult), reads=[p2, w1], writes=[p2])
                S.op(V_, lambda e: e.tensor_tensor(out=w1[:], in0=w1[:], in1=gw[:], op=ALU.mult), reads=[w1, gw], writes=[w1])
                S.op(V_, lambda e: e.tensor_tensor(out=p2[:], in0=p2[:], in1=gw[:], op=ALU.mult), reads=[p2, gw], writes=[p2])
                S.op(V_, lambda e: e.tensor_tensor(out=k1[:], in0=k1[:], in1=w1[:, 0:NB_].unsqueeze(2).to_broadcast([128, NB_, 8]), op=ALU.mult), reads=[k1, w1], writes=[k1])
                S.op(V_, lambda e: e.tensor_tensor(out=k2[:], in0=k2[:], in1=p2[:, 0:NB_].unsqueeze(2).to_broadcast([128, NB_, 8]), op=ALU.mult), reads=[k2, p2], writes=[k2])
                S.op(V_, lambda e: e.tensor_tensor(out=ge[:], in0=k1[:], in1=k2[:], op=ALU.add), reads=[k1, k2], writes=[ge])
                for g in range(4):
                    S.op(V_, lambda e, g=g: e.tensor_tensor(out=gates[:, :, g * 8:(g + 1) * 8], in0=ge[:], in1=mg[:, :, g:g + 1].to_broadcast([128, NB_, 8]), op=ALU.mult),
                         reads=[ge, mg], writes=[gates])

            def final_block(b):
                o, oap = ot[b % 2]
                norm_block((hb[b], hb[b][:]), gfb, (o, oap), ssts[b % 2])
                S.dma('sync', lambda e, oap=oap, b=b: e.dma_start(out=out_d[b * 128:(b + 1) * 128, :], in_=oap), reads=[o], is_output=True)

            def expert_tb(e_, tb, last):
                i = e_ % 2
                wg_t, wu_t, wd_t = wgb[i], wub[i], wdb[i]
                gates = gts[tb]
                tsl = slice(tb * TBK, (tb + 1) * TBK)
                for m in range(4):
                    pG = nps()
                    for k in range(8):
                        S.op(P_, lambda e, k=k, m=m, pG=pG: e.matmul(pG[:, 0:TBK], lhsT=wg_t[:, k, m * 128:(m + 1) * 128], rhs=n2T[:, k, tsl],
                                                                     start=(k == 0), stop=(k == 7)), reads=[wg_t, n2T], writes=[pG], inc=(k == 7))
                    pU_ = nps()
                    for k in range(8):
                        S.op(P_, lambda e, k=k, m=m, pU_=pU_: e.matmul(pU_[:, 0:TBK], lhsT=wu_t[:, k, m * 128:(m + 1) * 128], rhs=n2T[:, k, tsl],
                                                                       start=(k == 0), stop=(k == 7)), reads=[wu_t, n2T], writes=[pU_], inc=(k == 7))
                    sgt = sgs[m % 2]
                    S.op(A_, lambda e, pG=pG, sgt=sgt: e.activation(out=sgt[:], in_=pG[:, 0:TBK], func=AF.Silu), reads=[pG], writes=[sgt])
                    S.op(V_, lambda e, pU_=pU_, sgt=sgt, m=m: e.tensor_tensor(out=hT[:, m, tsl], in0=sgt[:], in1=pU_[:, 0:TBK], op=ALU.mult),
                         reads=[sgt, pU_], writes=[hT])
                for tt in range(NBt):
                    bi = tb * NBt + tt
                    for half in range(2):
                        pD = nps()
                        for m in range(4):
                            S.op(P_, lambda e, m=m, pD=pD, bi=bi, half=half: e.matmul(
                                pD[:, :], lhsT=hT[:, m, bi * 128:(bi + 1) * 128], rhs=wd_t[:, m, half * 512:(half + 1) * 512],
                                start=(m == 0), stop=(m == 3)), reads=[hT, wd_t], writes=[pD], inc=(m == 3))
                        hap = hb[bi][:, half * 512:(half + 1) * 512]
                        S.op(V_, lambda e, pD=pD, hap=hap, tt=tt: e.scalar_tensor_tensor(
                            out=hap, in0=pD[:, :], scalar=gates[:, tt, e_:e_ + 1], in1=hap, op0=ALU.mult, op1=ALU.add),
                            reads=[pD, gates, hb[bi]], writes=[hb[bi]])
                    if last:
                        final_block(bi)

            for tb in range(NTB):
                front(tb)
                gating(tb)
                if n_exp > 0:
                    expert_tb(0, tb, n_exp == 1)
            if n_exp > 2:
                load_expert(2)
            for e_ in range(1, n_exp):
                for tb in range(NTB):
                    expert_tb(e_, tb, e_ == n_exp - 1)
                if e_ + 2 < n_exp:
                    load_expert(e_ + 2)
            if n_exp == 0:
                for b in range(NB):
                    final_block(b)
            S.finish('sync')
            S.emit()
    return nc


def feature_major(v, nchunk):
    return np.ascontiguousarray(np.asarray(v, np.float32).reshape(nchunk, 128).T)


def make_in_maps(inputs, T):
    I = {k: np.asarray(v) for k, v in inputs.items()}
    B = I['x'].shape[0]
    shared = {
        'norm1_g': I['norm1_g'].reshape(1, D), 'norm2_g': I['norm2_g'].reshape(1, D), 'final_g': I['final_norm_g'].reshape(1, D),
        'w_in': I['w_in'][0], 'w_out': I['w_out'][0],
        'mu_l': feature_major(I['rwkv_mu'][0], 14),
        'rvec_l': np.ascontiguousarray(np.stack([feature_major(I[k][0].reshape(-1), 4) for k in
                                                 ('rwkv_w0', 'rwkv_a0', 'rwkv_k_k', 'rwkv_k_a', 'rwkv_r_k')], axis=1)),
        'wa_up': np.ascontiguousarray(np.concatenate([I['rwkv_w_up'][0], I['rwkv_a_up'][0]], axis=0)),
        'g_up': I['rwkv_g_up'][0],
        'ln_w': I['rwkv_ln_w'].reshape(1, 512), 'ln_b': I['rwkv_ln_b'].reshape(1, 512), 'hg_norm': I['hgrn_norm_g'].reshape(1, 512),
        'conv_l': np.ascontiguousarray(I['hgrn_conv_w'][0].reshape(4, 12, 128).transpose(2, 0, 1)),
        'lb_l': np.ascontiguousarray(I['hgrn_lb_logits'].reshape(2, 4, 128).transpose(2, 0, 1)),
        'w_router': np.ascontiguousarray(np.concatenate([I['router_g_w'][0], I['router_e_w'][0]], axis=1)),
        'b_router': np.ascontiguousarray(np.concatenate([I['router_g_b'][0], I['router_e_b'][0]], axis=0).reshape(1, 36)),
        'e_gate': I['exp_w_gate'][0], 'e_up': I['exp_w_up'][0], 'e_down': I['exp_w_down'][0],
    }
    shared = {k: np.ascontiguousarray(v, dtype=np.float32) for k, v in shared.items()}
    maps = []
    for c in range(B):
        m = dict(shared)
        m['x'] = np.ascontiguousarray(I['x'][c, :T], dtype=np.float32)
        maps.append(m)
    return maps


def kernel(**inputs):
    x = np.asarray(inputs['x'])
    B, T, _ = x.shape
    nc = build(T)
    in_maps = make_in_maps(inputs, T)
    res = run_bass_kernel_spmd(nc, in_maps, core_ids=list(range(B)))
    return np.stack([np.asarray(r['out'], dtype=np.float32) for r in res.results], axis=0)
```

```python
import contextlib
import numpy as np
import concourse.bass as bass
import concourse.mybir as mybir
from concourse.bass_utils import run_bass_kernel_spmd

F32 = mybir.dt.float32
BF16 = mybir.dt.bfloat16
ALU = mybir.AluOpType
AF = mybir.ActivationFunctionType
AX = mybir.AxisListType

D = 1024
NE = 32
DE = 512
C0 = float(np.exp(-0.5))
RMS_EPS = 1e-6
GN_EPS = 64e-5


class Tk:
    def __init__(self, name, handle):
        self.name = name
        self.t = handle
        self.lw = None
        self.rd = []
        self.dsem = None
        self.dcnt = 0
        self.tw = 0.0
        self.tr = 0.0

    def __getitem__(self, idx):
        return self.t[idx]


class _Rec:
    def __init__(self):
        self.call = None

    def __getattr__(self, name):
        def f(*a, **k):
            self.call = (name, a, k)
            return self
        return f


def _record(fn):
    r = _Rec()
    fn(r)
    assert r.call is not None
    return r.call


class Sched:
    ENG = ['tensor', 'vector', 'scalar', 'gpsimd', 'sync']

    def __init__(self, nc, stack):
        self.nc = nc
        self.stack = stack
        self.sem = {e: stack.enter_context(nc.semaphore('s_' + e)) for e in self.ENG}
        self.cnt = {e: 0 for e in self.ENG}
        self.waited = {e: {} for e in self.ENG}
        self.ops = {e: [] for e in self.ENG}
        self.ntile = 0
        self.out_tokens = []
        self.all_dma = {}
        self.psr = 0
        self.rec = None
        self.pspool = (0, 8)
        self.psrs = {}
        self.tm = {e: 0.0 for e in self.ENG}

    def sb(self, name, shape, dtype=F32, stack=None):
        self.ntile += 1
        h = (stack or self.stack).enter_context(self.nc.sbuf_tensor(f'{name}_{self.ntile}', list(shape), dtype))
        return Tk(name, h)

    def ps(self, name, shape, dtype=F32, stack=None):
        self.ntile += 1
        h = (stack or self.stack).enter_context(self.nc.psum_tensor(f'{name}_{self.ntile}', list(shape), dtype))
        return Tk(name, h)

    def _deps(self, eng, reads, writes):
        deps = {}

        def add(tok):
            if tok is None:
                return
            s, v = tok
            if deps.get(s, 0) < v:
                deps[s] = v
        for t in reads:
            add(t.lw)
        for t in writes:
            add(t.lw)
            for r in t.rd:
                add(r)
        waits = []
        for s, v in deps.items():
            if eng == 'tensor' and s is self.sem['tensor']:
                continue
            if self.waited[eng].get(s, 0) >= v:
                continue
            self.waited[eng][s] = v
            waits.append((s, v))
        return waits

    def record(self, f, pspool=(0, 4)):
        assert self.rec is None
        self.rec = []
        old = self.pspool
        self.pspool = pspool
        f()
        out, self.rec = self.rec, None
        self.pspool = old
        return out

    @staticmethod
    def _dur(kind, eng, call):
        if kind == 'dma':
            return 2.0
        name, a, k = call
        out = k.get('out', a[0] if a else None)
        try:
            n = int(np.prod(out.shape[1:]))
        except Exception:
            n = 128
        if eng == 'tensor':
            return 0.07 + n / 2200.0
        if eng == 'vector':
            return 0.10 + n / 960.0
        if eng == 'scalar':
            return 0.25 + n / 1200.0
        return 0.25 + n / 480.0

    def _est(self, item):
        kind, args = item
        eng, call, reads, writes = args[0], args[1], args[2], args[3]
        dep = 0.0
        for t in reads:
            dep = max(dep, t.tw)
        for t in writes:
            dep = max(dep, t.tw, t.tr)
        start = max(self.tm[eng], dep + 0.4)
        return start, start + self._dur(kind, eng, call)

    def _tcommit(self, kind, eng, call, reads, writes):
        start, fin = self._est((kind, (eng, call, reads, writes)))
        self.tm[eng] = start + (0.05 if kind == 'dma' else fin - start)
        for t in reads:
            t.tr = max(t.tr, fin)
        for t in writes:
            t.tw = fin
            t.tr = 0.0

    def run_interleaved(self, la, lb):
        na, nb = len(la), len(lb)
        i = j = 0
        while i < na or j < nb:
            if j >= nb:
                pick_a = True
            elif i >= na:
                pick_a = False
            else:
                pick_a = self._est(la[i])[0] < self._est(lb[j])[0]
            if pick_a:
                kind, args = la[i]; i += 1
            else:
                kind, args = lb[j]; j += 1
            getattr(self, kind)(*args)

    def op(self, eng, fn, reads=(), writes=(), inc=True):
        if self.rec is not None:
            self.rec.append(('op', (eng, _record(fn), tuple(reads), tuple(writes), inc)))
            return
        waits = self._deps(eng, reads, writes)
        call = fn if isinstance(fn, tuple) else _record(fn)
        fn = call
        self._tcommit('op', eng, call, reads, writes)
        tok = (self.sem[eng], self.cnt[eng] + 1)
        if inc:
            self.cnt[eng] += 1
        for t in reads:
            t.rd.append(tok)
        for t in writes:
            t.lw = tok
            t.rd = []
        self.ops[eng].append((fn if isinstance(fn, tuple) else _record(fn), waits, (self.sem[eng], 1) if inc else None))

    def dma(self, eng, fn, reads=(), writes=(), is_output=False):
        if self.rec is not None:
            self.rec.append(('dma', (eng, _record(fn), tuple(reads), tuple(writes), is_output)))
            return
        waits = self._deps(eng, reads, writes)
        call = fn if isinstance(fn, tuple) else _record(fn)
        fn = call
        self._tcommit('dma', eng, call, reads, writes)
        owner = (list(writes) + list(reads))[0]
        if owner.dsem is None:
            self.ntile += 1
            owner.dsem = self.stack.enter_context(self.nc.semaphore(f'd_{owner.name}_{self.ntile}'))
        owner.dcnt += 16
        tok = (owner.dsem, owner.dcnt)
        self.all_dma[id(owner.dsem)] = tok
        for t in reads:
            t.rd.append(tok)
        for t in writes:
            t.lw = tok
            t.rd = []
        if is_output:
            self.out_tokens.append(tok)
        self.ops[eng].append((fn if isinstance(fn, tuple) else _record(fn), waits, (owner.dsem, 16)))

    def barrier(self):
        toks = [(self.sem[e], self.cnt[e]) for e in self.ENG if self.cnt[e] > 0] + list(self.all_dma.values())
        for e in self.ENG:
            waits = []
            for s, v in toks:
                if e == 'tensor' and s is self.sem['tensor']:
                    continue
                if self.waited[e].get(s, 0) >= v:
                    continue
                self.waited[e][s] = v
                waits.append((s, v))
            if waits:
                self.ops[e].append((None, waits, None))

    def finish(self, eng='sync'):
        deps = {}
        for s, v in self.out_tokens:
            deps[s] = max(deps.get(s, 0), v)
        self.ops[eng].append((None, list(deps.items()), None))

    def emit(self):
        nc = self.nc
        with nc.Block() as block:
            def run(engname):
                def body(e):
                    for fn, waits, inc in self.ops[engname]:
                        for s, v in waits:
                            e.wait_ge(s, v)
                        if fn is None:
                            continue
                        name, a, k = fn
                        ins = getattr(e, name)(*a, **k)
                        if inc is not None:
                            ins.then_inc(inc[0], inc[1])
                return body
            block.tensor(run('tensor'))
            block.vector(run('vector'))
            block.scalar(run('scalar'))
            block.gpsimd(run('gpsimd'))
            block.sync(run('sync'))


class Arena:
    def __init__(self, S, nwords):
        self.tk = S.sb('arena', [128, nwords])
        self.n = nwords
        self.off = 0

    def reset(self):
        self.off = 0

    def alloc(self, name, shape, dtype=F32):
        n = int(np.prod(shape[1:]))
        words = n if dtype is F32 else (n + 1) // 2
        assert self.off + words <= self.n, (name, self.off, words, self.n)
        ap = self.tk.t[:, self.off:self.off + words]
        if dtype is not F32:
            ap = ap.bitcast(dtype)
        if len(shape) == 3:
            ap = ap.rearrange('p (a b) -> p a b', b=shape[2])
        self.off += words
        return Tk(name, ap)


ARENA_WORDS = 131 * 256


def build(T, n_exp=NE, do_p1=True, do_p2=True, do_router=True, nb1=None, p1_stage=9):
    NB = T // 128
    nc = bass.Bass("TRN2", target_bir_lowering=False)

    def din(name, shape):
        return nc.dram_tensor(name, list(shape), F32, kind="ExternalInput").ap()
    x_d = din("x", [T, D])
    g1_d = din("norm1_g", [1, D]); g2_d = din("norm2_g", [1, D]); gf_d = din("final_g", [1, D])
    win_d = din("w_in", [D, 3840]); wout_d = din("w_out", [D, D])
    mu_d = din("mu_l", [128, 14])
    rv_d = din("rvec_l", [128, 5, 4])
    waup_d = din("wa_up", [128, 512]); gup_d = din("g_up", [128, 512])
    lnw_d = din("ln_w", [1, 512]); lnb_d = din("ln_b", [1, 512]); hng_d = din("hg_norm", [1, 512])
    cw_d = din("conv_l", [128, 4, 12]); lbl_d = din("lb_l", [128, 2, 4])
    wr_d = din("w_router", [D, 36]); br_d = din("b_router", [1, 36])
    wg_d = din("e_gate", [NE, D, DE]); wu_d = din("e_up", [NE, D, DE]); wd_d = din("e_down", [NE, DE, D])
    out_d = nc.dram_tensor("out", [T, D], F32, kind="ExternalOutput").ap()

    with contextlib.ExitStack() as st:
        S = Sched(nc, st)
        V_, A_, G_, P_ = 'vector', 'scalar', 'gpsimd', 'tensor'

        hb = [S.sb(f'h{b}', [128, D]) for b in range(NB)]
        g1b = S.sb('g1b', [128, D])
        ident = S.sb('ident', [128, 128]); identb = S.sb('identb', [128, 128], BF16)
        MU = S.sb('MU', [128, 128]); MUs = S.sb('MUs', [128, 128]); MLs = S.sb('MLs', [128, 128])
        smask = S.sb('smask', [128, 128]); hsel = S.sb('hsel', [128, 2]); bones = S.sb('bones', [128, 128])
        psb = [S.ps(f'pb{i}', [128, 512]) for i in range(8)]
        AR = Arena(S, ARENA_WORDS)

        def nps():
            lo, hi = S.pspool
            r = S.psrs.get((lo, hi), lo - 1) + 1
            if r >= hi:
                r = lo
            S.psrs[(lo, hi)] = r
            return psb[r]

        def ld(tk, src, eng='sync'):
            S.dma(eng, lambda e: e.dma_start(out=tk[:], in_=src), writes=[tk])

        ld(g1b, g1_d.partition_broadcast(128))
        for b in range(NB):
            S.dma('sync', lambda e, b=b: e.dma_start(out=hb[b][:], in_=x_d[b * 128:(b + 1) * 128, :]), writes=[hb[b]])

        S.op(G_, lambda e: e.memset(ident[:], 0.0), writes=[ident])
        S.op(G_, lambda e: e.affine_select(out=ident[:], in_=ident[:], compare_op=ALU.not_equal, fill=1.0, base=0,
                                           pattern=[[-1, 128]], channel_multiplier=1), reads=[ident], writes=[ident])
        S.op(V_, lambda e: e.tensor_copy(out=identb[:], in_=ident[:]), reads=[ident], writes=[identb])
        for m, cmp_, cm, pat, zb in ((MU, ALU.is_ge, -1, 1, (0, 64)), (MUs, ALU.is_gt, -1, 1, (0, 64)), (MLs, ALU.is_gt, 1, -1, (64, 0))):
            S.op(G_, lambda e, m=m: e.memset(m[:], 1.0), writes=[m])
            S.op(G_, lambda e, m=m, cmp_=cmp_, cm=cm, pat=pat: e.affine_select(
                out=m[:], in_=m[:], compare_op=cmp_, fill=0.0, base=0, pattern=[[pat, 128]], channel_multiplier=cm),
                reads=[m], writes=[m])
            S.op(G_, lambda e, m=m, zb=zb: e.memset(m[zb[0]:zb[0] + 64, zb[1]:zb[1] + 64], 0.0), reads=[m], writes=[m])
        S.op(V_, lambda e: e.memset(smask[:], 1.0), writes=[smask])
        S.op(V_, lambda e: e.memset(smask[:].rearrange('p (c j) -> p c j', j=64)[:, :, 0:1], 0.0), reads=[smask], writes=[smask])
        S.op(V_, lambda e: e.memset(hsel[:], 0.0), writes=[hsel])
        S.op(V_, lambda e: e.memset(hsel[0:64, 0:1], 1.0), reads=[hsel], writes=[hsel])
        S.op(V_, lambda e: e.memset(hsel[64:128, 1:2], 1.0), reads=[hsel], writes=[hsel])
        S.op(V_, lambda e: e.memset(bones[:], 0.0), writes=[bones])
        S.op(V_, lambda e: e.memset(bones[0:64, 0:64], 1.0), reads=[bones], writes=[bones])
        S.op(V_, lambda e: e.memset(bones[64:128, 64:128], 1.0), reads=[bones], writes=[bones])

        def norm_block(xt, gb, outt, sst, eng_sq=A_):
            xtk, xap = xt
            otk, oap = outt
            junk_tk, junk_ap = otk, oap
            s_tk = sst
            S.op(V_, lambda e: e.memset(s_tk[:, 0:1], 0.0), writes=[s_tk])
            S.op(A_, lambda e: e.activation(out=junk_ap, in_=xap, func=AF.Square, accum_out=s_tk[:, 0:1]),
                 reads=[xtk, s_tk], writes=[junk_tk, s_tk])
            S.op(V_, lambda e: e.tensor_scalar(out=s_tk[:, 1:2], in0=s_tk[:, 0:1], scalar1=1.0 / D, scalar2=RMS_EPS,
                                               op0=ALU.mult, op1=ALU.add), reads=[s_tk], writes=[s_tk])
            S.op(A_, lambda e: e.activation(out=s_tk[:, 1:2], in_=s_tk[:, 1:2], func=AF.Sqrt), reads=[s_tk], writes=[s_tk])
            S.op(V_, lambda e: e.reciprocal(out=s_tk[:, 1:2], in_=s_tk[:, 1:2]), reads=[s_tk], writes=[s_tk])
            S.op(V_, lambda e: e.scalar_tensor_tensor(out=oap, in0=xap, scalar=s_tk[:, 1:2], in1=gb[:],
                                                      op0=ALU.mult, op1=ALU.mult), reads=[xtk, s_tk, gb], writes=[otk])

        def transpose_to(dst_tk, dst_ap_fn, src_tk, src_aps, idt, dtype, evac_eng):
            n = len(src_aps)
            pb = nps()
            if dtype is BF16:
                pv = pb[:].bitcast(BF16)
            else:
                pv = pb[:]
            for i, sap in enumerate(src_aps):
                S.op(P_, lambda e, i=i, sap=sap: e.transpose(pv[:, i * 128:(i + 1) * 128], sap, idt[:]),
                     reads=[src_tk, idt], writes=[pb], inc=(i == n - 1))
            src = pv[:, 0:n * 128].rearrange('p (n j) -> p n j', j=128)
            if evac_eng == A_:
                S.op(A_, lambda e: e.copy(out=dst_ap_fn(n), in_=src), reads=[pb], writes=[dst_tk])
            else:
                S.op(evac_eng, lambda e: e.tensor_copy(out=dst_ap_fn(n), in_=src), reads=[pb], writes=[dst_tk])

        def load_norm_transpose(b, xts, nb_t, nT, sst, gb):
            if xts is None:
                xt = hb[b]
            else:
                xt = xts[b % 2]
                S.dma('sync', lambda e: e.dma_start(out=xt[:], in_=x_d[b * 128:(b + 1) * 128, :]), writes=[xt])
            norm_block((xt, xt[:]), gb, (nb_t, nb_t[:]), sst)
            for half in range(2):
                transpose_to(nT, lambda n, half=half: nT[:, half * 4:half * 4 + 4, :], nb_t,
                             [nb_t[:, (half * 4 + i) * 128:(half * 4 + i + 1) * 128] for i in range(4)], identb, BF16,
                             A_ if half == 0 else V_)
            return xt

        def outproj_add(b, y_tk, y_ap, wo, first, xt, scr_bf, yT):
            S.op(V_, lambda e: e.tensor_copy(out=scr_bf[:], in_=y_ap), reads=[y_tk], writes=[scr_bf])
            transpose_to(yT, lambda n: yT[:, 0:4, :], scr_bf, [scr_bf[:, i * 128:(i + 1) * 128] for i in range(4)], identb, BF16, A_)
            for half in range(2):
                pb = nps()
                for k in range(4):
                    S.op(P_, lambda e, k=k, half=half, pb=pb: e.matmul(pb[:, :], lhsT=yT[:, k, :], rhs=wo[:, k, half * 512:(half + 1) * 512],
                                                                      start=(k == 0), stop=(k == 3)),
                         reads=[yT, wo], writes=[pb], inc=(k == 3))
                hap = hb[b][:, half * 512:(half + 1) * 512]
                S.op(V_, lambda e, pb=pb, hap=hap: e.tensor_tensor(out=hap, in0=pb[:, :], in1=hap, op=ALU.add),
                     reads=[pb, hb[b]], writes=[hb[b]])

        if True:
            sb = AR.alloc
            wr_in = sb('wr_in', [128, 8, 1792], BF16)
            wo1 = sb('wo1', [128, 4, D], BF16)
            S.dma(G_, lambda e: e.dma_start(out=wr_in[:], in_=win_d[:, 0:1792].rearrange('(k p) n -> p k n', p=128)), writes=[wr_in])
            S.dma(G_, lambda e: e.dma_start(out=wo1[:], in_=wout_d[0:512, :].rearrange('(k p) n -> p k n', p=128)), writes=[wo1])
            mu_t = sb('mu', [128, 14]); rv = sb('rv', [128, 5, 4]); omka = sb('omka', [128, 4])
            waup = sb('waup', [128, 512]); gup = sb('gup', [128, 512])
            lnw = sb('lnw', [128, 512]); lnb = sb('lnb', [128, 512])
            ld(mu_t, mu_d); ld(rv, rv_d); ld(waup, waup_d); ld(gup, gup_d)
            ld(lnw, lnw_d.partition_broadcast(128)); ld(lnb, lnb_d.partition_broadcast(128))
            S.op(V_, lambda e: e.tensor_scalar(out=omka[:], in0=rv[:, 3, :], scalar1=-1.0, scalar2=1.0, op0=ALU.mult, op1=ALU.add),
                 reads=[rv], writes=[omka])
            nb_t = sb('nb', [128, D], BF16); nT = sb('nT', [128, 8, 128], BF16)
            sst = sb('sst', [128, 2])
            pT1 = sb('pT', [128, 14, 129])
            pm = sb('pm', [128, 14, 128])
            th = sb('th', [128, 128]); sg = sb('sg', [128, 128])
            sw = sb('sw', [128, 4, 128]); cum = sb('cum', [128, 4, 128])
            E3 = sb('E3', [128, 4, 128]); aT = sb('aT', [128, 4, 128])
            kk = sb('kk', [128, 4, 128]); t4 = sb('t4', [128, 4, 128])
            SD = BF16
            bg = sb('bg', [128, 4, 128], SD); kg = sb('kg', [128, 4, 128], SD)
            E1 = sb('E1', [128, 4, 128]); E2 = sb('E2', [128, 4, 128])
            kmod = sw; rk = cum
            ifc1 = [(sb('ag', [128, 4, 128], SD), sb('rg', [128, 4, 128], SD), sb('Vt', [128, 512]), sb('Vtb', [128, 512], SD),
                     sb('bgT', [128, 512], SD), sb('kgT', [128, 512], SD), sb('gC', [128, 4, 2]), sb('gt', [128, 512]), sb('bsum', [128, 8]))
                    for _ in range(2)]
            Pm = [sb('P0', [128, 8, 128], SD), sb('P1', [128, 8, 128], SD)]
            PTm = [sb('PT0', [128, 8, 128], SD), sb('PT1', [128, 8, 128], SD)]
            Nm = Pm[1]; NTm = PTm[1]
            Rm1 = sb('R', [128, 8, 128], SD)
            Aak = sb('Aak', [128, 8, 128], SD); Arb = sb('Arb', [128, 8, 128], SD); Ark = sb('Ark', [128, 8, 128], SD)
            ST = sb('ST', [128, 4, 64]); STb = sb('STb', [128, 4, 128], SD); xs = sb('xs', [128, 8, 64], SD); us = sb('us', [128, 8, 64], SD)
            ysb = sb('ysb', [128, 8, 64]); yc = sb('yc', [128, 8, 64]); st8 = sb('st8', [128, 4, 8])
            ybf = sb('ybf', [128, 512], BF16); yT = sb('yT', [128, 4, 128], BF16)
            S.op(V_, lambda e: e.memset(ST[:], 0.0), writes=[ST])
            S.op(V_, lambda e: e.memset(STb[:], 0.0), writes=[STb])
            S.op(V_, lambda e: e.memset(pT1[:, :, 0:1], 0.0), writes=[pT1])

            def hrows(h):
                return slice((h % 2) * 64, (h % 2) * 64 + 64)

            def frontA(b):
                xt = load_norm_transpose(b, None, nb_t, nT, sst, g1b)
                cur = pT1
                for g0 in range(0, 14, 4):
                    n = min(4, 14 - g0)
                    pb = nps()
                    for i in range(n):
                        for k in range(8):
                            S.op(P_, lambda e, i=i, k=k, g0=g0, pb=pb: e.matmul(
                                pb[:, i * 128:(i + 1) * 128], lhsT=wr_in[:, k, (g0 + i) * 128:(g0 + i + 1) * 128], rhs=nT[:, k, :],
                                start=(k == 0), stop=(k == 7)), reads=[wr_in, nT], writes=[pb], inc=(k == 7 and i == n - 1))
                    src = pb[:, 0:n * 128].rearrange('p (n j) -> p n j', j=128)
                    if (g0 // 4) % 2 == 0:
                        S.op(A_, lambda e, src=src, g0=g0, n=n, cur=cur: e.copy(out=cur[:, g0:g0 + n, 1:129], in_=src), reads=[pb], writes=[cur])
                    else:
                        S.op(V_, lambda e, src=src, g0=g0, n=n, cur=cur: e.tensor_copy(out=cur[:, g0:g0 + n, 1:129], in_=src), reads=[pb], writes=[cur])
                S.op(V_, lambda e, cur=cur: e.tensor_tensor(out=pm[:], in0=cur[:, :, 0:128], in1=cur[:, :, 1:129], op=ALU.subtract),
                     reads=[cur], writes=[pm])
                S.op(V_, lambda e: e.tensor_tensor(out=pm[:], in0=pm[:], in1=mu_t[:, 0:14].unsqueeze(2).to_broadcast([128, 14, 128]), op=ALU.mult),
                     reads=[pm, mu_t], writes=[pm])
                S.op(V_, lambda e, cur=cur: e.tensor_tensor(out=pm[:], in0=pm[:], in1=cur[:, :, 1:129], op=ALU.add),
                     reads=[pm, cur], writes=[pm])
                S.op(G_, lambda e, cur=cur: e.tensor_copy(out=cur[:, :, 0:1], in_=cur[:, :, 128:129]), reads=[cur], writes=[cur])
                S.op(A_, lambda e: e.activation(out=th[0:64, :], in_=pm[0:64, 12, :], func=AF.Tanh), reads=[pm], writes=[th])
                S.op(A_, lambda e: e.activation(out=sg[:], in_=pm[:, 13, :], func=AF.Sigmoid), reads=[pm], writes=[sg])
                pz = nps()
                for c in range(4):
                    S.op(P_, lambda e, c=c, pz=pz: e.matmul(pz[:, c * 128:(c + 1) * 128], lhsT=waup[0:64, c * 128:(c + 1) * 128], rhs=th[0:64, :],
                                                            start=True, stop=True), reads=[waup, th], writes=[pz], inc=(c == 3))
                pa = nps()
                for c in range(4):
                    S.op(P_, lambda e, c=c, pa=pa: e.matmul(pa[:, c * 128:(c + 1) * 128], lhsT=waup[64:128, c * 128:(c + 1) * 128], rhs=pm[64:128, 12, :],
                                                            start=True, stop=True), reads=[waup, pm], writes=[pa], inc=(c == 3))
                for c in range(4):
                    S.op(A_, lambda e, c=c, pz=pz: e.activation(out=sw[:, c, :], in_=pz[:, c * 128:(c + 1) * 128], func=AF.Sigmoid, bias=rv[:, 0, c:c + 1]),
                         reads=[pz, rv], writes=[sw])
                    S.op(A_, lambda e, c=c, pa=pa: e.activation(out=aT[:, c, :], in_=pa[:, c * 128:(c + 1) * 128], func=AF.Sigmoid, bias=rv[:, 1, c:c + 1]),
                         reads=[pa, rv], writes=[aT])
                for c in range(4):
                    S.op(V_, lambda e, c=c: e.tensor_tensor_scan(out=cum[:, c, :], data0=smask[:], data1=sw[:, c, :], initial=0.0, op0=ALU.mult, op1=ALU.add),
                         reads=[smask, sw], writes=[cum])
                S.op(V_, lambda e: e.tensor_tensor(out=t4[:], in0=cum[:], in1=sw[:], op=ALU.subtract), reads=[cum, sw], writes=[t4])
                S.op(A_, lambda e: e.activation(out=E1[:], in_=cum[:], func=AF.Exp, scale=-C0), reads=[cum], writes=[E1])
                S.op(A_, lambda e: e.activation(out=E3[:], in_=cum[:], func=AF.Exp, scale=C0), reads=[cum], writes=[E3])
                S.op(A_, lambda e: e.activation(out=E2[:], in_=t4[:], func=AF.Exp, scale=-C0), reads=[t4], writes=[E2])
                S.op(V_, lambda e: e.tensor_tensor(out=kk[:], in0=pm[:, 4:8, :], in1=rv[:, 2, :].unsqueeze(2).to_broadcast([128, 4, 128]), op=ALU.mult),
                     reads=[pm, rv], writes=[kk])
                S.op(V_, lambda e: e.tensor_tensor(out=t4[:], in0=kk[:], in1=kk[:], op=ALU.mult), reads=[kk], writes=[t4])
                pn = nps()
                S.op(P_, lambda e, pn=pn: e.matmul(pn[:, :], lhsT=bones[:], rhs=t4[:].rearrange('p c j -> p (c j)'), start=True, stop=True),
                     reads=[bones, t4], writes=[pn])
                S.op(A_, lambda e, pn=pn: e.activation(out=t4[:].rearrange('p c j -> p (c j)'), in_=pn[:, :], func=AF.Sqrt), reads=[pn], writes=[t4])
                S.op(V_, lambda e: e.tensor_scalar_max(out=t4[:], in0=t4[:], scalar1=1e-12), reads=[t4], writes=[t4])
                S.op(V_, lambda e: e.reciprocal(out=t4[:], in_=t4[:]), reads=[t4], writes=[t4])
                S.op(V_, lambda e: e.tensor_tensor(out=kk[:], in0=kk[:], in1=t4[:], op=ALU.mult), reads=[kk, t4], writes=[kk])
                S.op(V_, lambda e: e.tensor_tensor(out=kmod[:], in0=aT[:], in1=rv[:, 3, :].unsqueeze(2).to_broadcast([128, 4, 128]), op=ALU.mult),
                     reads=[aT, rv], writes=[kmod])
                S.op(V_, lambda e: e.tensor_tensor(out=kmod[:], in0=kmod[:], in1=omka[:, 0:4].unsqueeze(2).to_broadcast([128, 4, 128]), op=ALU.add),
                     reads=[kmod, omka], writes=[kmod])
                S.op(V_, lambda e: e.tensor_tensor(out=kmod[:], in0=kmod[:], in1=pm[:, 4:8, :], op=ALU.mult), reads=[kmod, pm], writes=[kmod])

            def frontB(b):
                ag, rg, Vt, Vtb, bgT, kgT, gC, gt, bsum = ifc1[b % 2]
                pg = nps()
                S.op(P_, lambda e, pg=pg: e.matmul(pg[:, :], lhsT=sg[:], rhs=gup[:], start=True, stop=True), reads=[sg, gup], writes=[pg])
                S.op(V_, lambda e, pg=pg: e.tensor_copy(out=gt[:], in_=pg[:, :]), reads=[pg], writes=[gt])
                S.op(G_, lambda e: e.tensor_copy(out=gC[:], in_=E1[:].rearrange('p c (h j) -> p c h j', j=64)[:, :, :, 63]), reads=[E1], writes=[gC])
                S.op(V_, lambda e: e.scalar_tensor_tensor(out=ag[:], in0=kk[:], scalar=-1.0, in1=E2[:], op0=ALU.mult, op1=ALU.mult),
                     reads=[kk, E2], writes=[ag])
                S.op(V_, lambda e: e.tensor_tensor(out=bg[:], in0=kk[:], in1=aT[:], op=ALU.mult), reads=[kk, aT], writes=[bg])
                S.op(V_, lambda e: e.tensor_tensor(out=bg[:], in0=bg[:], in1=E3[:], op=ALU.mult), reads=[bg, E3], writes=[bg])
                S.op(G_, lambda e: e.tensor_tensor(out=kg[:], in0=kmod[:], in1=E3[:], op=ALU.mult), reads=[kmod, E3], writes=[kg])
                S.op(V_, lambda e: e.tensor_tensor(out=rg[:], in0=pm[:, 0:4, :], in1=E1[:], op=ALU.mult), reads=[pm, E1], writes=[rg])
                S.op(G_, lambda e: e.tensor_tensor(out=rk[:], in0=pm[:, 0:4, :], in1=kmod[:], op=ALU.mult), reads=[pm, kmod], writes=[rk])
                S.op(G_, lambda e: e.tensor_tensor(out=rk[:], in0=rk[:], in1=rv[:, 4, :].unsqueeze(2).to_broadcast([128, 4, 128]), op=ALU.mult),
                     reads=[rk, rv], writes=[rk])
                transpose_to(Vt, lambda n: Vt[:].rearrange('p (n j) -> p n j', j=128), pm, [pm[:, 8 + i, :] for i in range(4)], ident, F32, A_)
                S.op(A_, lambda e: e.copy(out=Vtb[:], in_=Vt[:]), reads=[Vt], writes=[Vtb])
                transpose_to(bgT, lambda n: bgT[:].rearrange('p (n j) -> p n j', j=128), bg, [bg[:, i, :] for i in range(4)], identb, BF16, V_)
                transpose_to(kgT, lambda n: kgT[:].rearrange('p (n j) -> p n j', j=128), kg, [kg[:, i, :] for i in range(4)], identb, BF16, A_)
                pbn = nps()
                for c in range(4):
                    S.op(P_, lambda e, c=c, pbn=pbn: e.matmul(pbn[:, 2 * c:2 * c + 2], lhsT=rk[:, c, :], rhs=hsel[:], start=True, stop=True),
                         reads=[rk, hsel], writes=[pbn], inc=(c == 3))
                S.op(V_, lambda e, pbn=pbn: e.tensor_copy(out=bsum[:], in_=pbn[:, 0:8]), reads=[pbn], writes=[bsum])

            def frontC(b):
                ag, rg, Vt, Vtb, bgT, kgT, gC, gt, bsum = ifc1[b % 2]
                import os
                _nd = int(os.environ.get('P1_ND', '5')); _nomask = os.environ.get('P1_NOMASK') == '1'; _nomm = os.environ.get('P1_NOMM') == '1'
                for (dst, lt, rt, mask) in ((Nm, bg, ag, MUs), (NTm, ag, bg, MLs), (Aak, kg, ag, MUs), (Arb, bg, rg, MU), (Ark, kg, rg, MU))[:_nd]:
                    for par in range(2):
                        pb = nps()
                        for hh in range(4):
                            h = 2 * hh + par
                            S.op(P_, lambda e, h=h, hh=hh, pb=pb, lt=lt, rt=rt: e.matmul(
                                pb[:, hh * 128:(hh + 1) * 128], lhsT=lt[hrows(h), h // 2, :], rhs=rt[hrows(h), h // 2, :], start=True, stop=True),
                                reads=[lt, rt], writes=[pb], inc=(hh == 3))
                        S.op(V_, lambda e, pb=pb, dst=dst, par=par, mask=mask: e.tensor_tensor(
                            out=dst[:, par:8:2, :], in0=pb[:, :].rearrange('p (n j) -> p n j', j=128),
                            in1=mask[:].unsqueeze(1).to_broadcast([128, 4, 128]), op=ALU.mult), reads=[pb, mask], writes=[dst])

            def back(b):
                ag, rg, Vt, Vtb, bgT, kgT, gC, gt, bsum = ifc1[b % 2]
                S.op(V_, lambda e: e.tensor_tensor(out=Rm1[:], in0=Nm[:], in1=ident[:].unsqueeze(1).to_broadcast([128, 8, 128]), op=ALU.add),
                     reads=[Nm, ident], writes=[Rm1])
                Pc, PTc, Rc = Nm, NTm, Rm1
                for lvl in range(5):
                    Pn, PTn, Rn = Pm[lvl % 2], PTm[lvl % 2], Rm1
                    last = (lvl == 4)
                    for hg in range(2):
                        hs = slice(hg * 4, hg * 4 + 4)
                        pbT = nps()
                        for hh in range(4):
                            h = hg * 4 + hh
                            S.op(P_, lambda e, h=h, hh=hh, pbT=pbT, Pc=Pc, PTc=PTc: e.matmul(
                                pbT[:, hh * 128:(hh + 1) * 128], lhsT=Pc[:, h, :], rhs=PTc[:, h, :], start=True, stop=True),
                                reads=[Pc, PTc], writes=[pbT], inc=(hh == 3))
                        S.op(A_, lambda e, pbT=pbT, PTn=PTn, hs=hs: e.copy(out=PTn[:, hs, :], in_=pbT[:, :].rearrange('p (n j) -> p n j', j=128)),
                             reads=[pbT], writes=[PTn])
                        if not last:
                            pbP = nps()
                            for hh in range(4):
                                h = hg * 4 + hh
                                S.op(P_, lambda e, h=h, hh=hh, pbP=pbP, Pc=Pc, PTc=PTc: e.matmul(
                                    pbP[:, hh * 128:(hh + 1) * 128], lhsT=PTc[:, h, :], rhs=Pc[:, h, :], start=True, stop=True),
                                    reads=[Pc, PTc], writes=[pbP], inc=(hh == 3))
                            S.op(V_, lambda e, pbP=pbP, Pn=Pn, hs=hs: e.tensor_copy(out=Pn[:, hs, :], in_=pbP[:, :].rearrange('p (n j) -> p n j', j=128)),
                                 reads=[pbP], writes=[Pn])
                        pbR = nps()
                        for hh in range(4):
                            h = hg * 4 + hh
                            S.op(P_, lambda e, h=h, hh=hh, pbR=pbR, PTn=PTn, Rc=Rc: e.matmul(
                                pbR[:, hh * 128:(hh + 1) * 128], lhsT=PTn[:, h, :], rhs=Rc[:, h, :], start=True, stop=True),
                                reads=[PTn, Rc], writes=[pbR], inc=(hh == 3))
                        S.op(V_, lambda e, pbR=pbR, Rn=Rn, Rc=Rc, hs=hs: e.tensor_tensor(
                            out=Rn[:, hs, :], in0=pbR[:, :].rearrange('p (n j) -> p n j', j=128), in1=Rc[:, hs, :], op=ALU.add),
                            reads=[pbR, Rc], writes=[Rn])
                    Pc, PTc, Rc = Pn, PTn, Rn
                Rf = Rc
                pY = nps()
                for cc in range(2):
                    cr = slice(cc * 64, cc * 64 + 64)
                    pX = nps()
                    for p in range(4):
                        for h in (2 * p, 2 * p + 1):
                            hc = slice(h * 64, h * 64 + 64)
                            S.op(P_, lambda e, p=p, h=h, hc=hc, cr=cr, pX=pX: e.matmul(pX[cr, hc], lhsT=ag[:, p, cr], rhs=STb[:, p, (h % 2) * 64:(h % 2) * 64 + 64],
                                                                                       start=True, stop=False), reads=[ag, STb], writes=[pX], inc=False)
                            S.op(P_, lambda e, h=h, hc=hc, cr=cr, pX=pX: e.matmul(pX[cr, hc], lhsT=Aak[cr, h, cr], rhs=Vtb[cr, hc],
                                                                                  start=False, stop=True), reads=[Aak, Vtb], writes=[pX], inc=(h == 7))
                    S.op(A_, lambda e, cr=cr, pX=pX: e.copy(out=xs[cr, :, :], in_=pX[cr, :].rearrange('p (h j) -> p h j', j=64)), reads=[pX], writes=[xs])
                    pU = nps()
                    for h in range(8):
                        hc = slice(h * 64, h * 64 + 64)
                        S.op(P_, lambda e, h=h, hc=hc, cr=cr, pU=pU: e.matmul(pU[cr, hc], lhsT=Rf[cr, h, cr], rhs=xs[cr, h, :], start=True, stop=True),
                             reads=[Rf, xs], writes=[pU], inc=(h == 7))
                    S.op(V_, lambda e, cr=cr, pU=pU: e.tensor_copy(out=us[cr, :, :], in_=pU[cr, :].rearrange('p (h j) -> p h j', j=64)), reads=[pU], writes=[us])
                    for p in range(4):
                        for h in (2 * p, 2 * p + 1):
                            hc = slice(h * 64, h * 64 + 64)
                            S.op(P_, lambda e, p=p, h=h, hc=hc, cr=cr: e.matmul(pY[cr, hc], lhsT=rg[:, p, cr], rhs=STb[:, p, (h % 2) * 64:(h % 2) * 64 + 64],
                                                                                start=True, stop=False), reads=[rg, STb], writes=[pY], inc=False)
                            S.op(P_, lambda e, h=h, hc=hc, cr=cr: e.matmul(pY[cr, hc], lhsT=Arb[cr, h, cr], rhs=us[cr, h, :], start=False, stop=False),
                                 reads=[Arb, us], writes=[pY], inc=False)
                            S.op(P_, lambda e, h=h, hc=hc, cr=cr: e.matmul(pY[cr, hc], lhsT=Ark[cr, h, cr], rhs=Vtb[cr, hc], start=False, stop=True),
                                 reads=[Ark, Vtb], writes=[pY], inc=(h == 7))
                    pS = nps()
                    for h in range(8):
                        hc = slice(h * 64, h * 64 + 64)
                        oc = slice((h // 2) * 64, (h // 2) * 64 + 64)
                        S.op(P_, lambda e, h=h, hc=hc, cr=cr, oc=oc, pS=pS: e.matmul(pS[hrows(h), oc], lhsT=bgT[cr, hc], rhs=us[cr, h, :], start=True, stop=False),
                             reads=[bgT, us], writes=[pS], inc=False)
                        S.op(P_, lambda e, h=h, hc=hc, cr=cr, oc=oc, pS=pS: e.matmul(pS[hrows(h), oc], lhsT=kgT[cr, hc], rhs=Vtb[cr, hc], start=False, stop=True),
                             reads=[kgT, Vtb], writes=[pS], inc=(h == 7))
                    S.op(V_, lambda e, pS=pS: e.tensor_tensor(out=ST[:], in0=pS[:, 0:256].rearrange('p (n j) -> p n j', j=64), in1=ST[:], op=ALU.add),
                         reads=[pS, ST], writes=[ST])
                    S.op(V_, lambda e, cc=cc: e.tensor_tensor(out=ST[:], in0=ST[:], in1=gC[:, :, cc:cc + 1].to_broadcast([128, 4, 64]), op=ALU.mult),
                         reads=[ST, gC], writes=[ST])
                    S.op(V_, lambda e: e.tensor_tensor(out=STb[:].rearrange('p c (t v) -> p c t v', t=2),
                                                       in0=ST[:].unsqueeze(2).to_broadcast([128, 4, 2, 64]),
                                                       in1=hsel[:].unsqueeze(1).unsqueeze(3).to_broadcast([128, 4, 2, 64]), op=ALU.mult),
                         reads=[ST, hsel], writes=[STb])
                S.op(A_, lambda e: e.copy(out=ysb[:], in_=pY[:, :].rearrange('p (h j) -> p h j', j=64)), reads=[pY], writes=[ysb])
                S.op(V_, lambda e: e.tensor_reduce(out=st8[:, 0, :], in_=ysb[:], axis=AX.X, op=ALU.add), reads=[ysb], writes=[st8])
                S.op(V_, lambda e: e.tensor_scalar(out=st8[:, 0, :], in0=st8[:, 0, :], scalar1=1.0 / 64, scalar2=None, op0=ALU.mult), reads=[st8], writes=[st8])
                S.op(V_, lambda e: e.tensor_tensor(out=yc[:], in0=ysb[:], in1=st8[:, 0, :].unsqueeze(2).to_broadcast([128, 8, 64]), op=ALU.subtract),
                     reads=[ysb, st8], writes=[yc])
                S.op(V_, lambda e: e.tensor_tensor(out=ysb[:], in0=yc[:], in1=yc[:], op=ALU.mult), reads=[yc], writes=[ysb])
                S.op(V_, lambda e: e.tensor_reduce(out=st8[:, 1, :], in_=ysb[:], axis=AX.X, op=ALU.add), reads=[ysb], writes=[st8])
                S.op(V_, lambda e: e.tensor_scalar(out=st8[:, 1, :], in0=st8[:, 1, :], scalar1=1.0 / 64, scalar2=GN_EPS, op0=ALU.mult, op1=ALU.add),
                     reads=[st8], writes=[st8])
                S.op(A_, lambda e: e.activation(out=st8[:, 1, :], in_=st8[:, 1, :], func=AF.Sqrt), reads=[st8], writes=[st8])
                S.op(V_, lambda e: e.reciprocal(out=st8[:, 1, :], in_=st8[:, 1, :]), reads=[st8], writes=[st8])
                S.op(V_, lambda e: e.tensor_tensor(out=yc[:], in0=yc[:], in1=st8[:, 1, :].unsqueeze(2).to_broadcast([128, 8, 64]), op=ALU.mult),
                     reads=[yc, st8], writes=[yc])
                ycf = yc[:].rearrange('p h j -> p (h j)')
                S.op(V_, lambda e: e.tensor_tensor(out=ycf, in0=ycf, in1=lnw[:], op=ALU.mult), reads=[yc, lnw], writes=[yc])
                S.op(V_, lambda e: e.tensor_tensor(out=ycf, in0=ycf, in1=lnb[:], op=ALU.add), reads=[yc, lnb], writes=[yc])
                S.op(V_, lambda e: e.tensor_tensor(out=ysb[:], in0=Vt[:].rearrange('p (h j) -> p h j', j=64),
                                                   in1=bsum[:, 0:8].unsqueeze(2).to_broadcast([128, 8, 64]), op=ALU.mult), reads=[Vt, bsum], writes=[ysb])
                S.op(V_, lambda e: e.tensor_tensor(out=yc[:], in0=yc[:], in1=ysb[:], op=ALU.add), reads=[yc, ysb], writes=[yc])
                S.op(V_, lambda e: e.tensor_tensor(out=ycf, in0=ycf, in1=gt[:], op=ALU.mult), reads=[yc, gt], writes=[yc])
                outproj_add(b, yc, ycf, wo1, True, None, ybf, yT)

            if do_p1:
                frontA(0)
                frontB(0)
                frontC(0)
                for b in range(NB):
                    _la = S.record(lambda: (frontA(b + 1), frontB(b + 1))) if b + 1 < NB else []
                    _lb = S.record(lambda: back(b), pspool=(4, 8))
                    S.run_interleaved(_la, _lb)
                    if b + 1 < NB:
                        frontC(b + 1)


        S.barrier()
        AR.reset()
        if True:
            sb = AR.alloc
            wh_in = sb('wh_in', [128, 8, 2048], BF16)
            wo2 = sb('wo2', [128, 4, D], BF16)
            S.dma(G_, lambda e: e.dma_start(out=wh_in[:], in_=win_d[:, 1792:3840].rearrange('(k p) n -> p k n', p=128)), writes=[wh_in])
            S.dma(G_, lambda e: e.dma_start(out=wo2[:], in_=wout_d[512:1024, :].rearrange('(k p) n -> p k n', p=128)), writes=[wo2])
            cw = sb('cw', [128, 4, 12]); lbl = sb('lbl', [128, 2, 4]); lb = sb('lb', [128, 4]); oml = sb('oml', [128, 4])
            hng = sb('hng', [128, 512])
            ld(cw, cw_d); ld(lbl, lbl_d); ld(hng, hng_d.partition_broadcast(128))
            S.op(V_, lambda e: e.tensor_tensor(out=lb[:], in0=lbl[:, 0, :], in1=lbl[:, 1, :], op=ALU.subtract), reads=[lbl], writes=[lb])
            S.op(A_, lambda e: e.activation(out=lb[:], in_=lb[:], func=AF.Sigmoid), reads=[lb], writes=[lb])
            S.op(V_, lambda e: e.tensor_scalar(out=oml[:], in0=lb[:], scalar1=-1.0, scalar2=1.0, op0=ALU.mult, op1=ALU.add), reads=[lb], writes=[oml])
            xts = [sb('xt0', [128, D]), sb('xt1', [128, D])]
            nb_t = sb('nb', [128, D], BF16); nT = sb('nT', [128, 8, 128], BF16)
            sst = sb('sst', [128, 2])
            ph1 = sb('ph', [128, 12, 131])
            cv = sb('cv', [128, 12, 128]); tmpcs = [sb('tc0', [128, 12, 128]), sb('tc1', [128, 12, 128])]
            cvi = Tk('cvi', cv.t); cvf = Tk('cvf', cv.t)
            phg = {0: ph1, 4: Tk('phf', ph1.t), 8: Tk('phi', ph1.t)}
            cvg = {0: cv, 4: cvf, 8: cvi}
            tmg = {0: tmpcs[0], 4: Tk('tcf', tmpcs[0].t), 8: tmpcs[1]}
            qs = sb('qs', [128, 4, 128]); fg = sb('fg', [128, 4, 128]); lf = sb('lf', [128, 4, 128]); bcum = sb('bcum', [128, 4, 128])
            eb = sb('eb', [128, 4, 128]); enb = sb('enb', [128, 4, 128]); kt = sb('kt', [128, 4, 128])
            SH = sb('SH', [128, 4, 128])
            ifc = [(sb('qt', [128, 4, 128]), sb('eC', [128, 4, 2]), sb('vI', [128, 512]), sb('ktT', [128, 512]), sb('gs', [128, 512]), sb('sc', [128, 4, 128]))
                   for _ in range(2)]
            osb = sb('osb', [128, 4, 128]); osq = sb('osq', [128, 4, 128]); st4 = sb('st4', [128, 4])
            ybf = sb('ybf', [128, 512], BF16); yT = sb('yT', [128, 4, 128], BF16)
            S.op(V_, lambda e: e.memset(SH[:], 0.0), writes=[SH])
            S.op(V_, lambda e: e.memset(ph1[:, :, 0:3], 0.0), writes=[phg[0], phg[4], phg[8]])

            def front2(b):
                qt, eC, vI, ktT, gs, sc = ifc[b % 2]
                xt = load_norm_transpose(b, xts, nb_t, nT, sst, g1b)
                cur = ph1
                for g0 in (4, 0, 8):
                    pb = nps()
                    for i in range(4):
                        for k in range(8):
                            S.op(P_, lambda e, i=i, k=k, g0=g0, pb=pb: e.matmul(
                                pb[:, i * 128:(i + 1) * 128], lhsT=wh_in[:, k, (g0 + i) * 128:(g0 + i + 1) * 128], rhs=nT[:, k, :],
                                start=(k == 0), stop=(k == 7)), reads=[wh_in, nT], writes=[pb], inc=(k == 7 and i == 3))
                    src = pb[:, :].rearrange('p (n j) -> p n j', j=128)
                    if g0 != 0:
                        S.op(A_, lambda e, src=src, g0=g0, cur=cur: e.copy(out=cur[:, g0:g0 + 4, 3:131], in_=src), reads=[pb], writes=[phg[g0]])
                    else:
                        S.op(V_, lambda e, src=src, g0=g0, cur=cur: e.tensor_copy(out=cur[:, g0:g0 + 4, 3:131], in_=src), reads=[pb], writes=[phg[g0]])
                pgt = nps()
                for k in range(8):
                    S.op(P_, lambda e, k=k, pgt=pgt: e.matmul(pgt[:, :], lhsT=nT[:, k, :], rhs=wh_in[:, k, 1536:2048], start=(k == 0), stop=(k == 7)),
                         reads=[nT, wh_in], writes=[pgt], inc=(k == 7))
                S.op(A_, lambda e, pgt=pgt: e.activation(out=gs[:], in_=pgt[:, :], func=AF.Silu), reads=[pgt], writes=[gs])
                for (eng, c0) in ((V_, 4), (G_, 8), (V_, 0)):
                    c1 = c0 + 4
                    phx, cvx, tm = phg[c0], cvg[c0], tmg[c0]
                    for w in range(4):
                        cwb = cw[:, w, c0:c1].unsqueeze(2).to_broadcast([128, 4, 128])
                        if w == 0:
                            S.op(eng, lambda e, cur=cur, cwb=cwb, c0=c0, c1=c1: e.tensor_tensor(out=cv[:, c0:c1, :], in0=cur[:, c0:c1, 0:128], in1=cwb, op=ALU.mult),
                                 reads=[phx, cw], writes=[cvx])
                        else:
                            S.op(eng, lambda e, cur=cur, cwb=cwb, w=w, tm=tm, c0=c0, c1=c1: e.tensor_tensor(out=tm[:, c0:c1, :], in0=cur[:, c0:c1, w:w + 128], in1=cwb, op=ALU.mult),
                                 reads=[phx, cw], writes=[tm])
                            S.op(eng, lambda e, tm=tm, c0=c0, c1=c1: e.tensor_tensor(out=cv[:, c0:c1, :], in0=cv[:, c0:c1, :], in1=tm[:, c0:c1, :], op=ALU.add),
                                 reads=[cvx, tm], writes=[cvx])
                S.op(G_, lambda e, cur=cur: e.tensor_copy(out=cur[:, :, 0:3], in_=cur[:, :, 128:131]), reads=[phg[0], phg[4], phg[8]], writes=[phg[0], phg[4], phg[8]])
                S.op(A_, lambda e: e.activation(out=fg[:], in_=cv[:, 4:8, :], func=AF.Sigmoid), reads=[cvf], writes=[fg])
                S.op(V_, lambda e: e.tensor_tensor(out=fg[:], in0=fg[:], in1=oml[:, 0:4].unsqueeze(2).to_broadcast([128, 4, 128]), op=ALU.mult),
                     reads=[fg, oml], writes=[fg])
                S.op(V_, lambda e: e.tensor_tensor(out=fg[:], in0=fg[:], in1=lb[:, 0:4].unsqueeze(2).to_broadcast([128, 4, 128]), op=ALU.add),
                     reads=[fg, lb], writes=[fg])
                S.op(A_, lambda e: e.activation(out=lf[:], in_=fg[:], func=AF.Ln), reads=[fg], writes=[lf])
                S.op(A_, lambda e: e.activation(out=qs[:], in_=cv[:, 0:4, :], func=AF.Silu), reads=[cv], writes=[qs])
                for c in range(4):
                    S.op(V_, lambda e, c=c: e.tensor_tensor_scan(out=bcum[:, c, :], data0=smask[:], data1=lf[:, c, :], initial=0.0, op0=ALU.mult, op1=ALU.add),
                         reads=[smask, lf], writes=[bcum])
                S.op(A_, lambda e: e.activation(out=eb[:], in_=bcum[:], func=AF.Exp), reads=[bcum], writes=[eb])
                S.op(A_, lambda e: e.activation(out=enb[:], in_=bcum[:], func=AF.Exp, scale=-1.0), reads=[bcum], writes=[enb])
                S.op(G_, lambda e: e.tensor_copy(out=eC[:], in_=eb[:].rearrange('p c (h j) -> p c h j', j=64)[:, :, :, 63]), reads=[eb], writes=[eC])
                S.op(V_, lambda e: e.tensor_tensor(out=qt[:], in0=qs[:], in1=eb[:], op=ALU.mult), reads=[qs, eb], writes=[qt])
                S.op(V_, lambda e: e.tensor_scalar(out=kt[:], in0=fg[:], scalar1=-1.0, scalar2=1.0, op0=ALU.mult, op1=ALU.add), reads=[fg], writes=[kt])
                S.op(V_, lambda e: e.tensor_tensor(out=kt[:], in0=kt[:], in1=enb[:], op=ALU.mult), reads=[kt, enb], writes=[kt])
                transpose_to(vI, lambda n: vI[:].rearrange('p (n j) -> p n j', j=128), cvi, [cv[:, 8 + i, :] for i in range(4)], ident, F32, A_)
                transpose_to(ktT, lambda n: ktT[:].rearrange('p (n j) -> p n j', j=128), kt, [kt[:, i, :] for i in range(4)], ident, F32, V_)
                psc = nps()
                for h in range(4):
                    S.op(P_, lambda e, h=h, psc=psc: e.matmul(psc[:, h * 128:(h + 1) * 128], lhsT=kt[:, h, :], rhs=qt[:, h, :], start=True, stop=True),
                         reads=[kt, qt], writes=[psc], inc=(h == 3))
                S.op(V_, lambda e, psc=psc: e.tensor_tensor(out=sc[:], in0=psc[:, :].rearrange('p (n j) -> p n j', j=128),
                                                            in1=MU[:].unsqueeze(1).to_broadcast([128, 4, 128]), op=ALU.mult), reads=[psc, MU], writes=[sc])

            def back2(b):
                qt, eC, vI, ktT, gs, sc = ifc[b % 2]
                pO = nps()
                for cc in range(2):
                    cr = slice(cc * 64, cc * 64 + 64)
                    for h in range(4):
                        hc = slice(h * 128, h * 128 + 128)
                        S.op(P_, lambda e, h=h, hc=hc, cr=cr: e.matmul(pO[cr, hc], lhsT=sc[cr, h, cr], rhs=vI[cr, hc], start=True, stop=False),
                             reads=[sc, vI], writes=[pO], inc=False)
                        S.op(P_, lambda e, h=h, hc=hc, cr=cr: e.matmul(pO[cr, hc], lhsT=qt[:, h, cr], rhs=SH[:, h, :], start=False, stop=True),
                             reads=[qt, SH], writes=[pO], inc=(h == 3))
                    pS = nps()
                    for h in range(4):
                        hc = slice(h * 128, h * 128 + 128)
                        S.op(P_, lambda e, h=h, hc=hc, cr=cr, pS=pS: e.matmul(pS[:, hc], lhsT=ktT[cr, hc], rhs=vI[cr, hc], start=True, stop=True),
                             reads=[ktT, vI], writes=[pS], inc=(h == 3))
                    S.op(V_, lambda e, pS=pS: e.tensor_tensor(out=SH[:], in0=pS[:, :].rearrange('p (n j) -> p n j', j=128), in1=SH[:], op=ALU.add),
                         reads=[pS, SH], writes=[SH])
                    S.op(V_, lambda e, cc=cc: e.tensor_tensor(out=SH[:], in0=SH[:], in1=eC[:, :, cc:cc + 1].to_broadcast([128, 4, 128]), op=ALU.mult),
                         reads=[SH, eC], writes=[SH])
                S.op(A_, lambda e: e.copy(out=osb[:], in_=pO[:, :].rearrange('p (h j) -> p h j', j=128)), reads=[pO], writes=[osb])
                S.op(V_, lambda e: e.tensor_tensor(out=osq[:], in0=osb[:], in1=osb[:], op=ALU.mult), reads=[osb], writes=[osq])
                S.op(V_, lambda e: e.tensor_reduce(out=st4[:], in_=osq[:], axis=AX.X, op=ALU.add), reads=[osq], writes=[st4])
                S.op(V_, lambda e: e.tensor_scalar(out=st4[:], in0=st4[:], scalar1=1.0 / 128, scalar2=RMS_EPS, op0=ALU.mult, op1=ALU.add), reads=[st4], writes=[st4])
                S.op(A_, lambda e: e.activation(out=st4[:], in_=st4[:], func=AF.Sqrt), reads=[st4], writes=[st4])
                S.op(V_, lambda e: e.reciprocal(out=st4[:], in_=st4[:]), reads=[st4], writes=[st4])
                S.op(V_, lambda e: e.tensor_tensor(out=osb[:], in0=osb[:], in1=st4[:, 0:4].unsqueeze(2).to_broadcast([128, 4, 128]), op=ALU.mult),
                     reads=[osb, st4], writes=[osb])
                osf = osb[:].rearrange('p h j -> p (h j)')
                S.op(V_, lambda e: e.tensor_tensor(out=osf, in0=osf, in1=hng[:], op=ALU.mult), reads=[osb, hng], writes=[osb])
                S.op(V_, lambda e: e.tensor_tensor(out=osf, in0=osf, in1=gs[:], op=ALU.mult), reads=[osb, gs], writes=[osb])
                outproj_add(b, osb, osf, wo2, False, None, ybf, yT)

            if do_p2:
                front2(0)
                for b in range(NB):
                    _la = S.record(lambda: front2(b + 1)) if b + 1 < NB else []
                    _lb = S.record(lambda: back2(b), pspool=(4, 8))
                    S.run_interleaved(_la, _lb)

        S.barrier()
        AR.reset()
        if True:
            sb = AR.alloc
            TBK = min(512, T)
            NTB = T // TBK
            NBt = TBK // 128
            n2T = sb('n2T', [128, 8, T], BF16)
            n2Th = [Tk(f'n2T{t}', n2T.t) for t in range(T // min(512, T))]
            wr = sb('wr', [128, 8, 36]); brb = sb('brb', [128, 36])
            S.dma('sync', lambda e: e.dma_start(out=wr[:], in_=wr_d.rearrange('(k p) n -> p k n', p=128)), writes=[wr])
            ld(brb, br_d.partition_broadcast(128))
            wgb = [sb('wg0', [128, 8, DE], BF16), sb('wg1', [128, 8, DE], BF16)]
            wub = [sb('wu0', [128, 8, DE], BF16), sb('wu1', [128, 8, DE], BF16)]
            wdb = [sb('wd0', [128, 4, D], BF16), sb('wd1', [128, 4, D], BF16)]

            def load_expert(e_):
                i = e_ % 2
                S.dma(G_, lambda e: e.dma_start(out=wgb[i][:], in_=wg_d[e_].rearrange('(k p) n -> p k n', p=128)), writes=[wgb[i]])
                S.dma(G_, lambda e: e.dma_start(out=wub[i][:], in_=wu_d[e_].rearrange('(k p) n -> p k n', p=128)), writes=[wub[i]])
                S.dma(G_, lambda e: e.dma_start(out=wdb[i][:], in_=wd_d[e_].rearrange('(k p) n -> p k n', p=128)), writes=[wdb[i]])
            if n_exp > 0:
                load_expert(0)
            if n_exp > 1:
                load_expert(1)
            g2b = sb('g2b', [128, D]); gfb = sb('gfb', [128, D])
            ld(g2b, g2_d.partition_broadcast(128)); ld(gfb, gf_d.partition_broadcast(128))
            n2s = [sb('n2a', [128, D]), sb('n2b', [128, D])]; n2T32s = [sb('n2T32a', [128, 8, 128]), sb('n2T32b', [128, 8, 128])]
            ssts = [sb('ssta', [128, 2]), sb('sstb', [128, 2])]
            n2, n2T32, sst = n2s[0], n2T32s[0], ssts[0]
            lgs = [sb(f'lg{t}', [128, NBt, 36]) for t in range(NTB)]
            gts = [sb(f'gates{t}', [128, NBt, 32]) for t in range(NTB)]
            mg = sb('mg', [128, NBt, 4]); gmx = sb('gmx', [128, NBt]); gex = sb('gex', [128, NBt, 4]); gw = sb('gw', [128, NBt])
            le = sb('le', [128, NBt, 32]); el = sb('el', [128, NBt, 8]); m1 = sb('m1', [128, NBt]); k1 = sb('k1', [128, NBt, 8])
            el2 = sb('el2', [128, NBt, 8]); m2 = sb('m2', [128, NBt]); k2 = sb('k2', [128, NBt, 8]); p2 = sb('p2', [128, NBt]); w1 = sb('w1', [128, NBt])
            ge = sb('ge', [128, NBt, 8])
            hT = sb('hT', [128, 4, T], BF16)
            sgs = [sb('sg0', [128, TBK]), sb('sg1', [128, TBK])]
            ot = [(n2s[0], n2s[0][:]), (n2s[1], n2s[1][:])]

            def front(tb):
                lg = lgs[tb]
                for j in range(NBt):
                    b = tb * NBt + j
                    n2, n2T32, sst = n2s[b % 2], n2T32s[b % 2], ssts[b % 2]
                    norm_block((hb[b], hb[b][:]), g2b, (n2, n2[:]), sst)
                    for half in range(2):
                        pbx = nps()
                        for i in range(4):
                            S.op(P_, lambda e, i=i, pbx=pbx, half=half, n2=n2: e.transpose(pbx[:, i * 128:(i + 1) * 128], n2[:, (half * 4 + i) * 128:(half * 4 + i + 1) * 128], ident[:]),
                                 reads=[n2, ident], writes=[pbx], inc=(i == 3))
                        srcx = pbx[:, :].rearrange('p (n j) -> p n j', j=128)
                        S.op(A_, lambda e, srcx=srcx, half=half, n2T32=n2T32: e.copy(out=n2T32[:, half * 4:half * 4 + 4, :], in_=srcx), reads=[pbx], writes=[n2T32])
                        S.op(V_, lambda e, half=half, b=b, n2T32=n2T32: e.tensor_copy(out=n2T[:, half * 4:half * 4 + 4, b * 128:(b + 1) * 128], in_=n2T32[:, half * 4:half * 4 + 4, :]),
                             reads=[n2T32], writes=[n2Th[tb]])
                    pr = nps()
                    for k in range(8):
                        S.op(P_, lambda e, k=k, pr=pr: e.matmul(pr[:, 0:36], lhsT=n2T32[:, k, :], rhs=wr[:, k, :], start=(k == 0), stop=(k == 7)),
                             reads=[n2T32, wr], writes=[pr], inc=(k == 7))
                    S.op(V_, lambda e, j=j, pr=pr: e.tensor_tensor(out=lg[:, j, :], in0=pr[:, 0:36], in1=brb[:], op=ALU.add), reads=[pr, brb], writes=[lg])

            def gating(tb):
                lg = lgs[tb]; gates = gts[tb]
                NB_ = NBt
                lgg = lg[:, :, 0:4]
                S.op(V_, lambda e: e.tensor_reduce(out=gmx[:], in_=lgg, axis=AX.X, op=ALU.max), reads=[lg], writes=[gmx])
                S.op(V_, lambda e: e.tensor_tensor(out=mg[:], in0=lgg, in1=gmx[:, 0:NB_].unsqueeze(2).to_broadcast([128, NB_, 4]), op=ALU.is_equal),
                     reads=[lg, gmx], writes=[mg])
                S.op(V_, lambda e: e.tensor_tensor(out=gex[:], in0=lgg, in1=gmx[:, 0:NB_].unsqueeze(2).to_broadcast([128, NB_, 4]), op=ALU.subtract),
                     reads=[lg, gmx], writes=[gex])
                S.op(A_, lambda e: e.activation(out=gex[:], in_=gex[:], func=AF.Exp), reads=[gex], writes=[gex])
                S.op(V_, lambda e: e.tensor_reduce(out=gw[:], in_=gex[:], axis=AX.X, op=ALU.add), reads=[gex], writes=[gw])
                S.op(V_, lambda e: e.reciprocal(out=gw[:], in_=gw[:]), reads=[gw], writes=[gw])
                S.op(V_, lambda e: e.tensor_tensor(out=le[:].rearrange('p b (g j) -> p b g j', j=8), in0=lg[:, :, 4:36].rearrange('p b (g j) -> p b g j', j=8),
                                                   in1=mg[:].unsqueeze(3).to_broadcast([128, NB_, 4, 8]), op=ALU.mult), reads=[lg, mg], writes=[le])
                S.op(V_, lambda e: e.tensor_reduce(out=el[:], in_=le[:].rearrange('p b (g j) -> p b j g', j=8), axis=AX.X, op=ALU.add), reads=[le], writes=[el])
                S.op(V_, lambda e: e.tensor_reduce(out=m1[:], in_=el[:], axis=AX.X, op=ALU.max), reads=[el], writes=[m1])
                S.op(V_, lambda e: e.tensor_tensor(out=k1[:], in0=el[:], in1=m1[:, 0:NB_].unsqueeze(2).to_broadcast([128, NB_, 8]), op=ALU.is_equal),
                     reads=[el, m1], writes=[k1])
                S.op(V_, lambda e: e.scalar_tensor_tensor(out=el2[:], in0=k1[:], scalar=-1e30, in1=el[:], op0=ALU.mult, op1=ALU.add), reads=[k1, el], writes=[el2])
                S.op(V_, lambda e: e.tensor_reduce(out=m2[:], in_=el2[:], axis=AX.X, op=ALU.max), reads=[el2], writes=[m2])
                S.op(V_, lambda e: e.tensor_tensor(out=k2[:], in0=el2[:], in1=m2[:, 0:NB_].unsqueeze(2).to_broadcast([128, NB_, 8]), op=ALU.is_equal),
                     reads=[el2, m2], writes=[k2])
                S.op(V_, lambda e: e.tensor_tensor(out=p2[:], in0=m2[:], in1=m1[:], op=ALU.subtract), reads=[m1, m2], writes=[p2])
                S.op(A_, lambda e: e.activation(out=p2[:], in_=p2[:], func=AF.Exp), reads=[p2], writes=[p2])
                S.op(V_, lambda e: e.tensor_scalar(out=w1[:], in0=p2[:], scalar1=1.0, scalar2=None, op0=ALU.add), reads=[p2], writes=[w1])
                S.op(V_, lambda e: e.reciprocal(out=w1[:], in_=w1[:]), reads=[w1], writes=[w1])
                S.op(V_, lambda e: e.tensor_tensor(out=p2[:], in0=p2[:], in1=w1[:], op=ALU.mult), reads=[p2, w1], writes=[p2])
                S.op(V_, lambda e: e.tensor_tensor(out=w1[:], in0=w1[:], in1=gw[:], op=ALU.mult), reads=[w1, gw], writes=[w1])
                S.op(V_, lambda e: e.tensor_tensor(out=p2[:], in0=p2[:], in1=gw[:], op=ALU.mult), reads=[p2, gw], writes=[p2])
                S.op(V_, lambda e: e.tensor_tensor(out=k1[:], in0=k1[:], in1=w1[:, 0:NB_].unsqueeze(2).to_broadcast([128, NB_, 8]), op=ALU.mult), reads=[k1, w1], writes=[k1])
                S.op(V_, lambda e: e.tensor_tensor(out=k2[:], in0=k2[:], in1=p2[:, 0:NB_].unsqueeze(2).to_broadcast([128, NB_, 8]), op=ALU.mult), reads=[k2, p2], writes=[k2])
                S.op(V_, lambda e: e.tensor_tensor(out=ge[:], in0=k1[:], in1=k2[:], op=ALU.add), reads=[k1, k2], writes=[ge])
                for g in range(4):
                    S.op(V_, lambda e, g=g: e.tensor_tensor(out=gates[:, :, g * 8:(g + 1) * 8], in0=ge[:], in1=mg[:, :, g:g + 1].to_broadcast([128, NB_, 8]), op=ALU.mult),
                         reads=[ge, mg], writes=[gates])

            def final_block(b):
                o, oap = ot[b % 2]
                norm_block((hb[b], hb[b][:]), gfb, (o, oap), ssts[b % 2])
                S.dma('sync', lambda e, oap=oap, b=b: e.dma_start(out=out_d[b * 128:(b + 1) * 128, :], in_=oap), reads=[o], is_output=True)

            def expert_tb(e_, tb, last):
                i = e_ % 2
                wg_t, wu_t, wd_t = wgb[i], wub[i], wdb[i]
                gates = gts[tb]
                tsl = slice(tb * TBK, (tb + 1) * TBK)
                for m in range(4):
                    pG = nps()
                    for k in range(8):
                        S.op(P_, lambda e, k=k, m=m, pG=pG: e.matmul(pG[:, 0:TBK], lhsT=wg_t[:, k, m * 128:(m + 1) * 128], rhs=n2T[:, k, tsl],
                                                                     start=(k == 0), stop=(k == 7)), reads=[wg_t, n2Th[tb]], writes=[pG], inc=(k == 7))
                    pU_ = nps()
                    for k in range(8):
                        S.op(P_, lambda e, k=k, m=m, pU_=pU_: e.matmul(pU_[:, 0:TBK], lhsT=wu_t[:, k, m * 128:(m + 1) * 128], rhs=n2T[:, k, tsl],
                                                                       start=(k == 0), stop=(k == 7)), reads=[wu_t, n2Th[tb]], writes=[pU_], inc=(k == 7))
                    sgt = sgs[m % 2]
                    S.op(A_, lambda e, pG=pG, sgt=sgt: e.activation(out=sgt[:], in_=pG[:, 0:TBK], func=AF.Silu), reads=[pG], writes=[sgt])
                    S.op(V_, lambda e, pU_=pU_, sgt=sgt, m=m: e.tensor_tensor(out=hT[:, m, tsl], in0=sgt[:], in1=pU_[:, 0:TBK], op=ALU.mult),
                         reads=[sgt, pU_], writes=[hT])
                for tt in range(NBt):
                    bi = tb * NBt + tt
                    for half in range(2):
                        pD = nps()
                        for m in range(4):
                            S.op(P_, lambda e, m=m, pD=pD, bi=bi, half=half: e.matmul(
                                pD[:, :], lhsT=hT[:, m, bi * 128:(bi + 1) * 128], rhs=wd_t[:, m, half * 512:(half + 1) * 512],
                                start=(m == 0), stop=(m == 3)), reads=[hT, wd_t], writes=[pD], inc=(m == 3))
                        hap = hb[bi][:, half * 512:(half + 1) * 512]
                        S.op(V_, lambda e, pD=pD, hap=hap, tt=tt: e.scalar_tensor_tensor(
                            out=hap, in0=pD[:, :], scalar=gates[:, tt, e_:e_ + 1], in1=hap, op0=ALU.mult, op1=ALU.add),
                            reads=[pD, gates, hb[bi]], writes=[hb[bi]])
                    if last:
                        final_block(bi)

            front(0)
            gating(0)
            for tb in range(NTB):
                _la = S.record(lambda: (front(tb + 1), gating(tb + 1))) if tb + 1 < NTB else []
                _lb = S.record(lambda: expert_tb(0, tb, n_exp == 1), pspool=(4, 8)) if n_exp > 0 else []
                S.run_interleaved(_la, _lb)
            if n_exp > 2:
                load_expert(2)
            for e_ in range(1, n_exp):
                for tb in range(NTB):
                    expert_tb(e_, tb, e_ == n_exp - 1)
                if e_ + 2 < n_exp:
                    load_expert(e_ + 2)
            if n_exp == 0:
                for b in range(NB):
                    final_block(b)
            S.finish('sync')
            S.emit()
    return nc


def feature_major(v, nchunk):
    return np.ascontiguousarray(np.asarray(v, np.float32).reshape(nchunk, 128).T)


def make_in_maps(inputs, T):
    I = {k: np.asarray(v) for k, v in inputs.items()}
    B = I['x'].shape[0]
    shared = {
        'norm1_g': I['norm1_g'].reshape(1, D), 'norm2_g': I['norm2_g'].reshape(1, D), 'final_g': I['final_norm_g'].reshape(1, D),
        'w_in': I['w_in'][0], 'w_out': I['w_out'][0],
        'mu_l': feature_major(I['rwkv_mu'][0], 14),
        'rvec_l': np.ascontiguousarray(np.stack([feature_major(I[k][0].reshape(-1), 4) for k in
                                                 ('rwkv_w0', 'rwkv_a0', 'rwkv_k_k', 'rwkv_k_a', 'rwkv_r_k')], axis=1)),
        'wa_up': np.ascontiguousarray(np.concatenate([I['rwkv_w_up'][0], I['rwkv_a_up'][0]], axis=0)),
        'g_up': I['rwkv_g_up'][0],
        'ln_w': I['rwkv_ln_w'].reshape(1, 512), 'ln_b': I['rwkv_ln_b'].reshape(1, 512), 'hg_norm': I['hgrn_norm_g'].reshape(1, 512),
        'conv_l': np.ascontiguousarray(I['hgrn_conv_w'][0].reshape(4, 12, 128).transpose(2, 0, 1)),
        'lb_l': np.ascontiguousarray(I['hgrn_lb_logits'].reshape(2, 4, 128).transpose(2, 0, 1)),
        'w_router': np.ascontiguousarray(np.concatenate([I['router_g_w'][0], I['router_e_w'][0]], axis=1)),
        'b_router': np.ascontiguousarray(np.concatenate([I['router_g_b'][0], I['router_e_b'][0]], axis=0).reshape(1, 36)),
        'e_gate': I['exp_w_gate'][0], 'e_up': I['exp_w_up'][0], 'e_down': I['exp_w_down'][0],
    }
    shared = {k: np.ascontiguousarray(v, dtype=np.float32) for k, v in shared.items()}
    maps = []
    for c in range(B):
        m = dict(shared)
        m['x'] = np.ascontiguousarray(I['x'][c, :T], dtype=np.float32)
        maps.append(m)
    return maps


def kernel(**inputs):
    x = np.asarray(inputs['x'])
    B, T, _ = x.shape
    nc = build(T)
    in_maps = make_in_maps(inputs, T)
    res = run_bass_kernel_spmd(nc, in_maps, core_ids=list(range(B)))
    return np.stack([np.asarray(r['out'], dtype=np.float32) for r in res.results], axis=0)
```
